# Optimizing a Trainium2 kernel written in Bass

```python
import jax, jax.numpy as jnp
from jax import lax
import numpy as np

D_MODEL = 1024
BATCH = 8
SEQ = 4096
DEPTH = 1

MEM_LEN = 256
EPS = 1e-6
HEAD_DIM = 64
ATTN_WIDTH = D_MODEL // 2
ATTN_Q_HEADS = ATTN_WIDTH // HEAD_DIM
ATTN_KV_HEADS = 2
ATTN_REP = ATTN_Q_HEADS // ATTN_KV_HEADS
KV_WIDTH = ATTN_KV_HEADS * HEAD_DIM
WINDOW = 128
ATTN_BLOCK = 128
SGU_WIDTH = D_MODEL - ATTN_WIDTH
SGU_GROUP_DIM = 64
SGU_GROUPS = SGU_WIDTH // SGU_GROUP_DIM
SGU_CHUNK = 128
IN_WIDTH = ATTN_WIDTH + 2 * KV_WIDTH + 2 * SGU_WIDTH
X_HEADS = 4
X_HEAD_DIM = D_MODEL // X_HEADS
N_GROUPS = 4
EXPERTS_PER_GROUP = 8
N_EXPERTS = N_GROUPS * EXPERTS_PER_GROUP
TOP_K = 2
D_EXPERT = D_MODEL // 2
MOE_BLOCK = 256

kernel_name = "hymba_swa_sgu_memxattn_hmoe"


def rms_norm(x, gain):
    xf = x.astype(jnp.float32)
    y = xf * lax.rsqrt(jnp.mean(xf * xf, axis=-1, keepdims=True) + EPS)
    return (y * gain.astype(jnp.float32)).astype(x.dtype)


def sliding_window_attention(q, k, v, sinks):
    B, S = q.shape[:2]
    nb = S // ATTN_BLOCK
    qb = q.reshape(B, nb, ATTN_BLOCK, ATTN_KV_HEADS, ATTN_REP, HEAD_DIM).astype(jnp.float32)
    kb = k.reshape(B, nb, ATTN_BLOCK, ATTN_KV_HEADS, HEAD_DIM)
    vb = v.reshape(B, nb, ATTN_BLOCK, ATTN_KV_HEADS, HEAD_DIM)
    pad = ((0, 0), (1, 0), (0, 0), (0, 0), (0, 0))
    kwin = jnp.concatenate([jnp.pad(kb, pad)[:, :-1], kb], axis=2).astype(jnp.float32)
    vwin = jnp.concatenate([jnp.pad(vb, pad)[:, :-1], vb], axis=2).astype(jnp.float32)
    scores = jnp.einsum('bnqhrd,bnkhd->bnhrqk', qb, kwin) * (HEAD_DIM ** -0.5)
    qi = jnp.arange(ATTN_BLOCK)[:, None]
    kj = jnp.arange(2 * ATTN_BLOCK)[None, :]
    dist = qi + ATTN_BLOCK - kj
    band = (dist >= 0) & (dist < WINDOW)
    key_pos = jnp.arange(nb)[:, None, None] * ATTN_BLOCK + kj[None] - ATTN_BLOCK
    mask = band[None] & (key_pos >= 0)
    scores = jnp.where(mask[None, :, None, None], scores, jnp.finfo(jnp.float32).min)
    sink = sinks.astype(jnp.float32).reshape(ATTN_KV_HEADS, ATTN_REP)[None, None, :, :, None, None]
    m = jnp.maximum(jnp.max(scores, axis=-1, keepdims=True), sink)
    p = jnp.exp(scores - m)
    denom = jnp.sum(p, axis=-1, keepdims=True) + jnp.exp(sink - m)
    out = jnp.einsum('bnhrqk,bnkhd->bnqhrd', p / denom, vwin)
    return out.reshape(B, S, ATTN_WIDTH).astype(q.dtype)


def spatial_gating(u, v, sgu_norm, sgu_w, sgu_b):
    B, S = u.shape[:2]
    nc = S // SGU_CHUNK
    vc = rms_norm(v, sgu_norm).reshape(B, nc, SGU_CHUNK, SGU_GROUPS, SGU_GROUP_DIM)
    causal = jnp.tril(jnp.ones((SGU_CHUNK, SGU_CHUNK), dtype=bool))
    w = jnp.where(causal[None], sgu_w, jnp.zeros((), sgu_w.dtype))
    s = jnp.einsum('gij,bcjgd->bcigd', w, vc) + sgu_b.T[None, None, :, :, None]
    return u * s.reshape(B, S, SGU_WIDTH)


def memory_cross_attention(h, mem_n, w_cq, w_ck, w_cv, cq_norm, ck_norm, w_co):
    B, S = h.shape[:2]
    M = mem_n.shape[1]
    q = rms_norm((h @ w_cq).reshape(B, S, X_HEADS, X_HEAD_DIM), cq_norm)
    k = rms_norm((mem_n @ w_ck).reshape(B, M, X_HEADS, X_HEAD_DIM), ck_norm)
    v = (mem_n @ w_cv).reshape(B, M, X_HEADS, X_HEAD_DIM)
    scores = jnp.einsum('bshd,bmhd->bhsm', q.astype(jnp.float32), k.astype(jnp.float32)) * (X_HEAD_DIM ** -0.5)
    p = jax.nn.softmax(scores, axis=-1)
    out = jnp.einsum('bhsm,bmhd->bshd', p, v.astype(jnp.float32)).astype(h.dtype)
    return out.reshape(B, S, D_MODEL) @ w_co


def hierarchical_moe(h, w_router_group, w_router_expert, w1, w3, w2):
    B, S, D = h.shape
    T = B * S
    xt = h.reshape(T, D)
    xf = xt.astype(jnp.float32)
    p_group = jax.nn.softmax(xf @ w_router_group.astype(jnp.float32), axis=-1)
    g_idx = jnp.argmax(p_group, axis=-1)
    g_w = jnp.take_along_axis(p_group, g_idx[:, None], axis=1)[:, 0]
    local = jnp.einsum('td,gde->tge', xf, w_router_expert.astype(jnp.float32))
    local = jnp.take_along_axis(local, g_idx[:, None, None], axis=1)[:, 0]
    top_logit, top_idx = lax.top_k(local, TOP_K)
    gate = jax.nn.softmax(top_logit, axis=-1) * g_w[:, None]
    flat_e = (g_idx[:, None] * EXPERTS_PER_GROUP + top_idx).reshape(-1).astype(jnp.int32)
    flat_w = gate.reshape(-1)
    A = T * TOP_K
    order = jnp.argsort(flat_e)
    sorted_e = flat_e[order]
    counts = jnp.zeros((N_EXPERTS,), jnp.int32).at[flat_e].add(1)
    starts = jnp.cumsum(counts) - counts
    padded = (counts + MOE_BLOCK - 1) // MOE_BLOCK * MOE_BLOCK
    pad_end = jnp.cumsum(padded)
    pad_start = pad_end - padded
    dest = pad_start[sorted_e] + jnp.arange(A, dtype=jnp.int32) - starts[sorted_e]
    nb = -(-A // MOE_BLOCK) + N_EXPERTS
    P = nb * MOE_BLOCK
    slot_tok = jnp.full((P,), T, jnp.int32).at[dest].set((order // TOP_K).astype(jnp.int32))
    slot_w = jnp.zeros((P,), jnp.float32).at[dest].set(flat_w[order])
    block_e = jnp.minimum(jnp.searchsorted(pad_end, jnp.arange(nb, dtype=jnp.int32) * MOE_BLOCK, side='right'), N_EXPERTS - 1)
    x_pad = jnp.concatenate([xt, jnp.zeros((1, D), xt.dtype)], axis=0)
    xb = x_pad[slot_tok].reshape(nb, MOE_BLOCK, D)

    def run_block(args):
        xblk, e = args
        hid = jax.nn.silu(xblk @ w1[e]) * (xblk @ w3[e])
        return hid @ w2[e]

    yb = lax.map(run_block, (xb, block_e)).reshape(P, D)
    y = jnp.zeros((T + 1, D), jnp.float32).at[slot_tok].add(yb.astype(jnp.float32) * slot_w[:, None])[:T]
    return y.astype(h.dtype).reshape(B, S, D)


def setup_inputs(seed: int = 0) -> dict:
    key = jax.random.key(seed)
    ks = jax.random.split(key, 32)
    f32 = jnp.float32
    nrm = lambda k, shape, s: jax.random.normal(k, shape, f32) * s
    gain = lambda k, n: 1.0 + 0.02 * jax.random.normal(k, (n,), f32)
    return {
        "x": nrm(ks[0], (BATCH, SEQ, D_MODEL), 1.0),
        "mem": nrm(ks[1], (BATCH, MEM_LEN, D_MODEL), 1.0),
        "norm_mix": gain(ks[2], D_MODEL),
        "w_in": nrm(ks[3], (D_MODEL, IN_WIDTH), D_MODEL ** -0.5),
        "q_norm": gain(ks[4], HEAD_DIM),
        "k_norm": gain(ks[5], HEAD_DIM),
        "attn_sinks": nrm(ks[6], (ATTN_Q_HEADS,), 0.5),
        "sgu_norm": gain(ks[7], SGU_WIDTH),
        "sgu_w": nrm(ks[8], (SGU_GROUPS, SGU_CHUNK, SGU_CHUNK), SGU_CHUNK ** -0.5),
        "sgu_b": 1.0 + nrm(ks[9], (SGU_GROUPS, SGU_CHUNK), 0.1),
        "out_norm_attn": gain(ks[10], ATTN_WIDTH),
        "out_norm_sgu": gain(ks[11], SGU_WIDTH),
        "w_out": nrm(ks[12], (D_MODEL, D_MODEL), D_MODEL ** -0.5),
        "norm_cross": gain(ks[13], D_MODEL),
        "norm_mem": gain(ks[14], D_MODEL),
        "w_cq": nrm(ks[15], (D_MODEL, D_MODEL), D_MODEL ** -0.5),
        "w_ck": nrm(ks[16], (D_MODEL, D_MODEL), D_MODEL ** -0.5),
        "w_cv": nrm(ks[17], (D_MODEL, D_MODEL), D_MODEL ** -0.5),
        "cq_norm": gain(ks[18], X_HEAD_DIM),
        "ck_norm": gain(ks[19], X_HEAD_DIM),
        "w_co": nrm(ks[20], (D_MODEL, D_MODEL), D_MODEL ** -0.5),
        "norm_ffn": gain(ks[21], D_MODEL),
        "w_router_group": nrm(ks[22], (D_MODEL, N_GROUPS), D_MODEL ** -0.5),
        "w_router_expert": nrm(ks[23], (N_GROUPS, D_MODEL, EXPERTS_PER_GROUP), D_MODEL ** -0.5),
        "w1": nrm(ks[24], (N_EXPERTS, D_MODEL, D_EXPERT), D_MODEL ** -0.5),
        "w3": nrm(ks[25], (N_EXPERTS, D_MODEL, D_EXPERT), D_MODEL ** -0.5),
        "w2": nrm(ks[26], (N_EXPERTS, D_EXPERT, D_MODEL), D_EXPERT ** -0.5),
    }


def reference(x, mem, norm_mix, w_in, q_norm, k_norm, attn_sinks, sgu_norm, sgu_w, sgu_b,
              out_norm_attn, out_norm_sgu, w_out, norm_cross, norm_mem, w_cq, w_ck, w_cv,
              cq_norm, ck_norm, w_co, norm_ffn, w_router_group, w_router_expert, w1, w3, w2):
    B, S = x.shape[:2]
    mem_n = rms_norm(mem, norm_mem)
    for _ in range(DEPTH):
        h = rms_norm(x, norm_mix)
        proj = h @ w_in
        c0 = ATTN_WIDTH
        c1 = c0 + KV_WIDTH
        c2 = c1 + KV_WIDTH
        c3 = c2 + SGU_WIDTH
        q = rms_norm(proj[..., :c0].reshape(B, S, ATTN_Q_HEADS, HEAD_DIM), q_norm)
        k = rms_norm(proj[..., c0:c1].reshape(B, S, ATTN_KV_HEADS, HEAD_DIM), k_norm)
        v = proj[..., c1:c2].reshape(B, S, ATTN_KV_HEADS, HEAD_DIM)
        u = jax.nn.gelu(proj[..., c2:c3])
        g = jax.nn.gelu(proj[..., c3:])
        attn = sliding_window_attention(q, k, v, attn_sinks)
        sg = spatial_gating(u, g, sgu_norm, sgu_w, sgu_b)
        mixed = jnp.concatenate([rms_norm(attn, out_norm_attn), rms_norm(sg, out_norm_sgu)], axis=-1)
        x = x + mixed @ w_out
        x = x + memory_cross_attention(rms_norm(x, norm_cross), mem_n, w_cq, w_ck, w_cv, cq_norm, ck_norm, w_co)
        x = x + hierarchical_moe(rms_norm(x, norm_ffn), w_router_group, w_router_expert, w1, w3, w2)
    return x
```

```python
import numpy as np
from contextlib import ExitStack
import concourse.bass as bass
import concourse.mybir as mybir
from concourse.bass_utils import run_bass_kernel_spmd

F32 = mybir.dt.float32
BF16 = mybir.dt.bfloat16
I32 = mybir.dt.int32
U8 = mybir.dt.uint8
AF = mybir.ActivationFunctionType
ALU = mybir.AluOpType
AX = mybir.AxisListType

ENGS = ['pe', 'act', 'dve', 'pool', 'sp']
EPS = 1e-6
D = 1024
SEQ = 4096
NCORES = 8
NEXP = 32
CAP = 512
NEG = -30000.0
PSUM_TAGS = ('ps', 'p2', 'p2h', 'p2y')


class Chan:
    def __init__(self, sem):
        self.sem = sem
        self.count = 0


class _Op:
    __slots__ = ('eng', 'fn', 'deps', 'odeps', 'signal', 'chan', 'val', 'dur', 'seg', 'cval')

    def __init__(self):
        self.signal = False
        self.chan = None
        self.val = None


class Sched:
    def __init__(self, csem, chans):
        self.csem = csem
        self.chans = chans
        self.ops = {e: [] for e in ENGS}
        self.last_w = {}
        self.readers = {}
        self.nwaits = 0
        self.seg = 0
        self.dma_op = {}

    def op(self, eng, fn, reads=(), writes=(), chan=None, relax_same=False, dur=None):
        o = _Op()
        o.eng = eng
        o.fn = fn
        o.dur = dur if dur is not None else (2500.0 if chan is not None else 300.0)
        o.seg = self.seg
        o.odeps = []
        deps = set()
        for r in reads:
            t = self.last_w.get(r)
            if t is not None:
                deps.add(t)
            if isinstance(r, tuple) and r[0] in PSUM_TAGS:
                for t in self.readers.get(r, ()):
                    if not (t[0] == 'c' and t[1] == eng):
                        deps.add(t)
        for w in writes:
            t = self.last_w.get(w)
            if t is not None:
                deps.add(t)
            for t in self.readers.get(w, ()):
                deps.add(t)
        if chan is not None:
            chan.count += 16
            o.chan = chan
            o.cval = chan.count
            ticket = ('d', chan, chan.count)
            self.dma_op[(id(chan), chan.count)] = (eng, len(self.ops[eng]))
        else:
            ticket = ('c', eng, len(self.ops[eng]))
        final = []
        for d in deps:
            if d[0] == 'c':
                if d[1] == eng and chan is None and (eng == 'pe' or relax_same):
                    o.odeps.append(d)
                    continue
                self.ops[d[1]][d[2]].signal = True
            final.append(d)
        o.deps = final
        key = (ticket[0], ticket[1])
        for r in reads:
            lst = self.readers.setdefault(r, [])
            for n, t in enumerate(lst):
                if (t[0], t[1]) == key:
                    lst[n] = ticket
                    break
            else:
                lst.append(ticket)
        for w in writes:
            self.last_w[w] = ticket
            self.readers[w] = []
        self.ops[eng].append(o)
        return ticket

    def fence(self, skip_chans=(), keep=()):
        kept = {r: self.last_w[r] for r in keep if r in self.last_w}
        tickets = []
        for e in ENGS:
            idx = None
            for i in range(len(self.ops[e]) - 1, -1, -1):
                if self.ops[e][i].chan is None and self.ops[e][i].fn is not None:
                    idx = i
                    break
            if idx is not None:
                self.ops[e][idx].signal = True
                tickets.append(('c', e, idx))
        for c in self.chans:
            if c.count and c not in skip_chans:
                tickets.append(('d', c, c.count))
        for e in ENGS:
            o = _Op()
            o.eng = e
            o.fn = None
            o.deps = list(tickets)
            o.odeps = []
            o.dur = 0.0
            o.seg = self.seg
            self.ops[e].append(o)
        self.seg += 1
        self.last_w = dict(kept)
        self.readers = {}

    def reschedule(self, seg, window=40, hop=200.0):
        rng = {}
        for e in ENGS:
            idx = [i for i, o in enumerate(self.ops[e]) if o.seg == seg and o.fn is not None]
            if idx:
                assert idx == list(range(idx[0], idx[-1] + 1))
                rng[e] = (idx[0], idx[-1] + 1)
        done = {}
        pend = {e: list(range(*rng[e])) for e in rng}
        free = {e: 0.0 for e in rng}
        new_order = {e: [] for e in rng}

        def prod(d):
            if d[0] == 'c':
                return (d[1], d[2])
            return self.dma_op[(id(d[1]), d[2])]

        def start_of(e, i):
            o = self.ops[e][i]
            t = free[e]
            for d in o.deps:
                pe_, pi = prod(d)
                if pe_ in rng and rng[pe_][0] <= pi < rng[pe_][1]:
                    c = done.get((pe_, pi))
                    if c is None:
                        return None
                    t = max(t, c + (0.0 if pe_ == e else hop))
            for d in o.odeps:
                pe_, pi = d[1], d[2]
                if pe_ in rng and rng[pe_][0] <= pi < rng[pe_][1]:
                    c = done.get((pe_, pi))
                    if c is None:
                        return None
                    t = max(t, c)
            return t

        total = sum(len(v) for v in pend.values())
        for _ in range(total):
            best = None
            for e in pend:
                lst = pend[e]
                if not lst:
                    continue
                w = 1 if e in ('sp', 'pool') else window
                for pos in range(min(w, len(lst))):
                    st = start_of(e, lst[pos])
                    if st is None:
                        continue
                    key = (st, pos)
                    if best is None or key < best[0]:
                        best = (key, e, pos)
                    if st <= free[e]:
                        break
            assert best is not None, "scheduler deadlock"
            (st, _), e, pos = best
            i = pend[e].pop(pos)
            o = self.ops[e][i]
            done[(e, i)] = st + o.dur
            free[e] = st + (o.dur if o.chan is None else 60.0)
            new_order[e].append(i)
        remap = {}
        for e in rng:
            lo = rng[e][0]
            for newpos, i in enumerate(new_order[e]):
                remap[(e, i)] = lo + newpos
        for e in rng:
            lo, hi = rng[e]
            self.ops[e][lo:hi] = [self.ops[e][i] for i in new_order[e]]
        for key in list(self.dma_op.keys()):
            v = self.dma_op[key]
            if v in remap:
                self.dma_op[key] = (v[0], remap[v])

        def fix(d):
            if d[0] == 'c' and (d[1], d[2]) in remap:
                return ('c', d[1], remap[(d[1], d[2])])
            return d
        for e in ENGS:
            for o in self.ops[e]:
                o.deps = [fix(d) for d in o.deps]
                o.odeps = [fix(d) for d in o.odeps]
        self.sim_span = max(done.values()) if done else 0.0
        for e in ENGS:
            if e not in rng:
                continue
            lo, hi = rng[e]
            if hi < len(self.ops[e]) and self.ops[e][hi].fn is None:
                last = None
                for i in range(hi - 1, lo - 1, -1):
                    if self.ops[e][i].chan is None:
                        last = i
                        break
                for e2 in ENGS:
                    f = self.ops[e2][rng[e2][1]] if e2 in rng else None
                    if f is None or f.fn is not None:
                        continue
                    f.deps = [d for d in f.deps if not (d[0] == 'c' and d[1] == e and lo <= d[2] < hi)]
                    if last is not None:
                        f.deps.append(('c', e, last))
                if last is not None:
                    self.ops[e][last].signal = True

    def emit(self, block):
        for e in ENGS:
            c = 0
            for o in self.ops[e]:
                if o.signal:
                    c += 1
                    o.val = c

        def run(e, eng):
            waited = {}
            for o in self.ops[e]:
                need = {}
                for d in o.deps:
                    if d[0] == 'c':
                        sem = self.csem[d[1]]
                        val = self.ops[d[1]][d[2]].val
                    else:
                        sem = d[1].sem
                        val = d[2]
                    k = id(sem)
                    if k not in need or need[k][1] < val:
                        need[k] = (sem, val)
                for k, (sem, val) in need.items():
                    if waited.get(k, 0) >= val:
                        continue
                    eng.wait_ge(sem, val)
                    self.nwaits += 1
                    waited[k] = val
                if o.fn is None:
                    continue
                ins = o.fn(eng)
                if o.chan is not None:
                    ins.then_inc(o.chan.sem, 16)
                elif o.signal:
                    ins.then_inc(self.csem[e], 1)

        @block.tensor
        def _(eng):
            run('pe', eng)

        @block.scalar
        def _(eng):
            run('act', eng)

        @block.vector
        def _(eng):
            run('dve', eng)

        @block.gpsimd
        def _(eng):
            run('pool', eng)

        @block.sync
        def _(eng):
            run('sp', eng)


class Arena:
    def __init__(self, t, nbytes):
        self.t = t
        self.n = nbytes
        self.off = 0

    def reset(self, off=0):
        self.off = off

    def alloc(self, shape, dt):
        esz = 4 if dt in (F32, I32) else 2
        per = esz
        for d in shape[1:]:
            per *= d
        self.off = (self.off + 63) // 64 * 64
        assert self.off + per <= self.n, ("arena overflow", self.off, per, self.n)
        v = self.t[0:shape[0], self.off:self.off + per].bitcast(dt)
        self.off += per
        if len(shape) == 3:
            v = v.rearrange("p (a b) -> p a b", a=shape[1])
        elif len(shape) == 4:
            v = v.rearrange("p (a b c) -> p a b c", a=shape[1], b=shape[2])
        return v


def build(NT=32, phases=3):
    S = NT * 128
    nc = bass.Bass("TRN2", target_bir_lowering=False)
    dr = {}

    def din(name, shape, dt=F32):
        dr[name] = nc.dram_tensor(name, list(shape), dt, kind="ExternalInput").ap()
        return dr[name]

    x_d = din("x", [S, D])
    mem_d = din("mem", [256, D])
    for nm, n in [("norm_mix", 1024), ("q_norm", 64), ("k_norm", 64), ("attn_sinks", 8), ("sgu_norm", 512),
                  ("out_norm_attn", 512), ("out_norm_sgu", 512), ("norm_cross", 1024), ("norm_mem", 1024),
                  ("cq_norm", 256), ("ck_norm", 256), ("norm_ffn", 1024)]:
        din(nm, [n])
    din("w_in", [D, 1792]); din("w_out", [D, D]); din("w_cq", [D, D]); din("w_ck", [D, D])
    din("w_cv", [D, D]); din("w_co", [D, D])
    din("sgu_w", [8, 128, 128]); din("sgu_b", [8, 128])
    din("w_router_group", [D, 4]); din("w_router_expert", [4, D, 8])
    din("w1", [NEXP, D, 512]); din("w3", [NEXP, D, 512]); din("w2", [NEXP, 512, D])
    out_d = nc.dram_tensor("out", [S, D], F32, kind="ExternalOutput").ap()
    NSLOT = NEXP * CAP
    xs_d = nc.dram_tensor("xs_scr", [NSLOT, D], BF16, kind="Internal").ap()
    ys_d = nc.dram_tensor("ys_scr", [NSLOT, D], BF16, kind="Internal").ap()

    with ExitStack() as es:
        E = es.enter_context
        arena_t = E(nc.sbuf_tensor("arena", [128, 186 * 1024], U8))
        pers_t = E(nc.sbuf_tensor("pers", [128, 16 * 1024], U8))
        pp = [E(nc.psum_tensor("pp%d" % i, [128, 1024], F32)) for i in range(4)]
        csem = {e: E(nc.semaphore("c_" + e)) for e in ENGS}
        chans = [Chan(E(nc.semaphore("dma%d" % i))) for i in range(80)]
        block = E(nc.Block())
        s = Sched(csem, chans)
        ar = Arena(arena_t, 186 * 1024)
        pa = Arena(pers_t, 16 * 1024)
        chi = [0]

        regs = {}

        def bcreg(e):
            if 'bc' not in regs:
                regs['bc'] = e.to_reg(NSLOT - 1)
            return regs['bc']

        def newchan():
            c = chans[chi[0]]
            chi[0] += 1
            return c

        def nfree(ap):
            n = 1
            for d in ap.shape[1:]:
                n *= d
            return n

        def edur(eng, ap, extra=0.0):
            n = nfree(ap)
            if eng == 'act':
                return 220.0 + 1.04 * n + extra
            if eng == 'dve':
                return 120.0 + 1.04 * n + extra
            return 600.0 + 2.2 * n + extra

        def dma(eng, out, in_, reads, writes, chan, **kw):
            s.op(eng, lambda e: e.dma_start(out=out, in_=in_, **kw), reads, writes, chan=chan)

        def mm(out, lhsT, rhs, start, stop, reads, writes):
            s.op('pe', lambda e: e.matmul(out, lhsT=lhsT, rhs=rhs, start=start, stop=stop), reads, writes,
                 dur=64.0 + 0.6 * max(nfree(rhs), 64) * (4 if rhs.dtype == F32 else 1))

        def tr(out, in_, idt, reads, writes):
            s.op('pe', lambda e: e.transpose(out=out, in_=in_, identity=idt), reads, writes, dur=64.0 + 0.6 * 128)

        def act(out, in_, func, reads, writes, scale=None, bias=None, accum=None):
            kw = {}
            if scale is not None:
                kw['scale'] = scale
            if bias is not None:
                kw['bias'] = bias
            if accum is not None:
                kw['accum_out'] = accum
            s.op('act', lambda e: e.activation(out=out, in_=in_, func=func, **kw), reads, writes,
                 dur=edur('act', out, 100.0 if accum is not None else 0.0))

        def tt(eng, out, in0, in1, op, reads, writes):
            s.op(eng, lambda e: e.tensor_tensor(out=out, in0=in0, in1=in1, op=op), reads, writes, dur=edur(eng, out))

        def ts(eng, out, in0, s1, s2, op0, op1, reads, writes):
            if op1 is None:
                s.op(eng, lambda e: e.tensor_scalar(out=out, in0=in0, scalar1=s1, scalar2=None, op0=op0), reads, writes, dur=edur(eng, out))
            else:
                s.op(eng, lambda e: e.tensor_scalar(out=out, in0=in0, scalar1=s1, scalar2=s2, op0=op0, op1=op1), reads, writes, dur=edur(eng, out))

        def stt(out, in0, scalar, in1, op0, op1, reads, writes):
            s.op('dve', lambda e: e.scalar_tensor_tensor(out=out, in0=in0, scalar=scalar, in1=in1, op0=op0, op1=op1), reads, writes,
                 dur=edur('dve', out))

        def cp(eng, out, in_, reads, writes):
            s.op(eng, lambda e: e.tensor_copy(out=out, in_=in_), reads, writes, dur=edur(eng, out))

        def red(out, in_, op, reads, writes):
            s.op('dve', lambda e: e.tensor_reduce(out=out, in_=in_, axis=AX.X, op=op), reads, writes, dur=edur('dve', in_))

        def rstd_of(out, ss, n, tag, reads, writes, tmp):
            ts('pool', tmp, ss, 1.0 / n, EPS, ALU.mult, ALU.add, reads, [tag + '_t'])
            w = neghalf[0:out.shape[0], 0:out.shape[1]]
            tt('pool', out, tmp, w, ALU.pow, [tag + '_t', 'consts'], writes)

        def rstd_fast(out, ss, n, tag, reads, writes, tmp):
            act(tmp, ss, AF.Ln, reads + ['consts'], [tag + '_t'], scale=1.0 / n, bias=epsc[0:out.shape[0], 0:1])
            act(out, tmp, AF.Exp, [tag + '_t'], writes, scale=-0.5)

        ident = pa.alloc([128, 128], BF16)
        identf = pa.alloc([128, 128], F32)
        ones_bf = pa.alloc([128, 128], BF16)
        ustrict = pa.alloc([128, 128], BF16)
        mb_cur = pa.alloc([128, 512], BF16)
        mb_prev = pa.alloc([128, 512], BF16)
        neghalf = pa.alloc([128, 16], F32)
        epsc = pa.alloc([128, 2], F32)
        gsgu = pa.alloc([128, 512], F32)
        bfull = pa.alloc([128, 512], F32)
        esink = pa.alloc([128, 8], F32)
        gmix = pa.alloc([128, 8], F32)
        gout = pa.alloc([128, 8], F32)
        gcross = pa.alloc([128, 8], F32)
        gmem = pa.alloc([128, 8], F32)
        gffn = pa.alloc([128, 8], F32)
        gqk = pa.alloc([128, 2], F32)
        gckq = pa.alloc([128, 4], F32)
        btmp = pa.alloc([128, 8], F32)
        ones_col = pa.alloc([128, 2], BF16)
        slot_i = pa.alloc([128, NT, 2], I32)
        gate_f = pa.alloc([128, NT, 2], F32)
        cnt = pa.alloc([128, 32], F32)
        eoff = pa.alloc([128, 32], F32)
        small = pa.alloc([128, 384], F32)
        gffn_b = pa.alloc([128, 1024], F32)

        c0 = newchan()
        ld = []

        cparts = []

        def cload(out, in_, **kw):
            cparts.append(('cpart', len(cparts)))
            dma('sp', out, in_, [], [cparts[-1]], c0, **kw)

        s.op('pool', lambda e: e.memset(identf, 0.0), [], ['identf'])
        s.op('pool', lambda e: e.affine_select(out=identf, in_=identf, pattern=[[-1, 128]], compare_op=ALU.not_equal,
                                               fill=1.0, base=0, channel_multiplier=1), ['identf'], ['identf'])
        cp('dve', ident, identf, ['identf'], ['consts'])
        s.op('dve', lambda e: e.memset(ones_bf, 1.0), [], ['consts'])
        s.op('dve', lambda e: e.memset(ones_col, 1.0), [], ['consts'])
        s.op('dve', lambda e: e.memset(epsc, EPS), [], ['consts'])
        s.op('dve', lambda e: e.memset(cnt, 0.0), [], ['cnt'])
        ztmp = ar.alloc([128, 512], F32)
        otmp = ar.alloc([128, 128], F32)
        s.op('pool', lambda e: e.memset(ztmp, 0.0), [], ['ztmp'])
        s.op('pool', lambda e: e.memset(otmp, 1.0), [], ['otmp'])
        s.op('pool', lambda e: e.affine_select(out=mb_cur, in_=ztmp, pattern=[[0, 4], [1, 128]], compare_op=ALU.is_ge,
                                               fill=NEG, base=0, channel_multiplier=-1), ['ztmp'], ['consts'])
        s.op('pool', lambda e: e.affine_select(out=mb_prev, in_=ztmp, pattern=[[0, 4], [-1, 128]], compare_op=ALU.is_ge,
                                               fill=NEG, base=-1, channel_multiplier=1), ['ztmp'], ['consts'])
        s.op('pool', lambda e: e.affine_select(out=ustrict, in_=otmp, pattern=[[1, 128]], compare_op=ALU.is_ge,
                                               fill=0.0, base=-1, channel_multiplier=-1), ['otmp'], ['consts'])
        s.op('pool', lambda e: e.iota(eoff, pattern=[[CAP, 32]], base=0, channel_multiplier=0,
                                      allow_small_or_imprecise_dtypes=True), [], ['consts'])

        def pc(v, c=128):
            return v.rearrange("(c p) -> p c", p=c)

        nsc = dict(allow_slow_non_contiguous=True)
        cload(gmix, pc(dr["norm_mix"]), **nsc)
        cload(gout[:, 0:4], pc(dr["out_norm_attn"]), **nsc)
        cload(gout[:, 4:8], pc(dr["out_norm_sgu"]), **nsc)
        cload(gcross, pc(dr["norm_cross"]), **nsc)
        cload(gmem, pc(dr["norm_mem"]), **nsc)
        cload(gffn, pc(dr["norm_ffn"]), **nsc)
        cload(gqk[0:64, 0:1], dr["q_norm"].rearrange("(p o) -> p o", o=1))
        cload(gqk[0:64, 1:2], dr["k_norm"].rearrange("(p o) -> p o", o=1))
        cload(gckq[:, 0:2], pc(dr["cq_norm"]), **nsc)
        cload(gckq[:, 2:4], pc(dr["ck_norm"]), **nsc)
        cload(gsgu, dr["sgu_norm"].partition_broadcast(128))
        cload(esink, dr["attn_sinks"].partition_broadcast(128))
        cload(btmp, dr["sgu_b"].rearrange("g i -> i g"), **nsc)
        cload(gffn_b, dr["norm_ffn"].partition_broadcast(128))
        s.op('dve', lambda e: e.memset(neghalf, -0.5), cparts, ['consts'])
        tt('dve', gqk[0:64, 0:1], gqk[0:64, 0:1], gqk[0:64, 1:2], ALU.mult, ['consts'], ['consts'])
        tt('dve', gckq[:, 0:2], gckq[:, 0:2], gckq[:, 2:4], ALU.mult, ['consts'], ['consts'])
        act(esink, esink, AF.Exp, ['consts'], ['consts'])
        cp('dve', bfull.rearrange("p (g d) -> p g d", d=64), btmp.unsqueeze(2).broadcast_to([128, 8, 64]), ['consts'], ['consts'])

        NSLOTS_T = 3
        ar.reset(0)
        w_in = ar.alloc([128, 8, 1792], BF16)
        w_out = ar.alloc([128, 8, 1024], BF16)
        w_cq = ar.alloc([128, 8, 1024], BF16)
        w_co = ar.alloc([128, 8, 1024], BF16)
        kcT = ar.alloc([128, 8, 256], BF16)
        vc = ar.alloc([128, 2, 1024], BF16)
        wT = ar.alloc([128, 8, 128], BF16)
        wr = ar.alloc([128, 8, 36], F32)
        zt = ar.alloc([128, 1024], BF16)

        class PB:
            pass
        pbs = []
        slot_off = []
        for b in range(NSLOTS_T):
            slot_off.append(ar.off)
            P = PB()
            P.xres = ar.alloc([128, 1024], F32)
            P.tb = ar.alloc([128, 1024], BF16)
            P.tT = ar.alloc([128, 8, 128], BF16)
            P.ug = ar.alloc([128, 1024], F32)
            P.x2T = P.ug.rearrange("p (c n) -> p c n", c=8)
            P.sq = ar.alloc([128, 1024], F32)
            P.asg = ar.alloc([128, 1024], F32)
            P.PT = ar.alloc([128, 4, 512], BF16)
            P.PcT = P.PT.rearrange("p a b -> p (a b)")[:, 0:1024].rearrange("p (c n) -> p c n", c=8)
            P.qk = ar.alloc([128, 640], BF16)
            P.qT = ar.alloc([64, 1024], BF16)
            P.kT = ar.alloc([64, 256], BF16)
            P.vaug = ar.alloc([128, 2, 66], BF16)
            P.gn = ar.alloc([128, 512], BF16)
            P.rt = ar.alloc([128, 320], F32)
            P.Mb = ar.alloc([128, 32], BF16)
            P.sm = small[:, b * 128:(b + 1) * 128]
            P.b = b
            pbs.append(P)
        ar_end1 = ar.off
        ar.reset(slot_off[0])
        w_ck = ar.alloc([128, 8, 1024], BF16)
        w_cv = ar.alloc([128, 8, 1024], BF16)
        NSTG_W = 4
        stage = [ar.alloc([128, 1792], F32) for _ in range(NSTG_W)]
        m_f = ar.alloc([128, 1024], F32)
        m_b = ar.alloc([128, 1024], BF16)
        m_T = ar.alloc([128, 8, 128], BF16)
        m_sq = ar.alloc([128, 1024], F32)
        assert ar.off <= ar_end1, (ar.off, ar_end1)

        bptr = [0]

        def bank1():
            k = bptr[0] % 8
            bptr[0] += 1
            return k

        def bank2():
            if bptr[0] % 2:
                bptr[0] += 1
            k = bptr[0] % 8
            bptr[0] += 2
            return k

        def BK(k):
            return pp[k // 2][:, (k % 2) * 512:(k % 2) * 512 + 512]

        def BK2(k):
            return pp[k // 2][:, :]

        def PSR(k):
            return ('ps', k)

        xsz = [('xsz', j) for j in range(NSLOT // 1024)] if phases >= 2 else []
        if phases >= 2:
            zch = newchan()
            s.op('pool', lambda e: e.memset(zt, 0.0), [], ['zt'])
            for j in range(NSLOT // 1024):
                dma('act', xs_d[j * 1024:(j + 1) * 1024, :].rearrange("(a p) d -> p a d", p=128),
                    zt.unsqueeze(1).broadcast_to([128, 8, 1024]), ['zt'], [('xsz', j)], zch)

        stg_ch = [newchan() for _ in range(NSTG_W)]
        wl = [0]

        def load_w(dst, src, ncols, gain, tag):
            for c in range(8):
                j = wl[0] % NSTG_W
                wl[0] += 1
                st = stage[j][:, 0:ncols]
                dma('sp', st, src[c * 128:(c + 1) * 128, :], [], [('stage', j)], stg_ch[j])
                eng = ['act', 'dve'][wl[0] % 2]
                if gain is None:
                    if eng == 'act':
                        act(dst[:, c, :], st, AF.Copy, [('stage', j)], [tag])
                    else:
                        cp(eng, dst[:, c, :], st, [('stage', j)], [tag])
                elif eng == 'act':
                    act(dst[:, c, :], st, AF.Copy, [('stage', j), 'consts'], [tag], scale=gain[:, c:c + 1])
                else:
                    ts(eng, dst[:, c, :], st, gain[:, c:c + 1], None, ALU.mult, None, [('stage', j), 'consts'], [tag])

        load_w(w_ck, dr["w_ck"], 1024, gmem, 'w_ck')
        load_w(w_cv, dr["w_cv"], 1024, gmem, 'w_cv')
        load_w(w_in, dr["w_in"], 1792, gmix, 'w_in')
        load_w(w_out, dr["w_out"], 1024, gout, 'w_out')
        load_w(w_cq, dr["w_cq"], 1024, gcross, 'w_cq')
        load_w(w_co, dr["w_co"], 1024, None, 'w_co')
        wr_raw = stage[0][:, 0:288].rearrange("p (c n) -> p c n", c=8)
        dma('sp', wr_raw[:, :, 0:4], dr["w_router_group"].rearrange("(c p) n -> p c n", p=128), [], [('stage', 0)], stg_ch[0])
        for g in range(4):
            dma('sp', wr_raw[:, :, 4 + 8 * g:12 + 8 * g], dr["w_router_expert"][g].rearrange("(c p) n -> p c n", p=128),
                [], [('stage', 0)], stg_ch[0])
        tt('dve', wr, wr_raw, gffn.unsqueeze(2).broadcast_to([128, 8, 36]), ALU.mult,
           [('stage', 0), 'consts'], ['wr'])
        sw_f = stage[1][:, 0:1024].rearrange("p (g j) -> p g j", g=8)
        dma('sp', sw_f, dr["sgu_w"].rearrange("g i j -> i g j"), [], [('stage', 1)], stg_ch[1])
        cp('dve', m_b.rearrange("p (g j) -> p g j", g=8), sw_f, [('stage', 1)], ['m_b'])
        k0 = bank1()
        q0b = BK(k0).bitcast(BF16)
        for g in range(8):
            tr(q0b[:, g * 128:(g + 1) * 128], m_b[:, g * 128:(g + 1) * 128], ident, ['m_b', 'consts'], [PSR(k0)])
        cp('dve', wT.rearrange("p g i -> p (g i)"), q0b, [PSR(k0)], ['wT'])
        s.op('pool', lambda e: e.affine_select(out=wT, in_=wT, pattern=[[0, 8], [1, 128]], compare_op=ALU.is_ge,
                                               fill=0.0, base=0, channel_multiplier=-1), ['wT'], ['wT'])

        def transpose8(src_bf, dstT, src_reg, dst_reg, evac_eng):
            k = bank1()
            bk = BK(k).bitcast(BF16)
            srcs = list(src_reg) if isinstance(src_reg, list) else [src_reg]
            dsts = list(dst_reg) if isinstance(dst_reg, list) else [dst_reg]
            for c in range(8):
                tr(bk[:, c * 128:(c + 1) * 128], src_bf[:, c * 128:(c + 1) * 128], ident, srcs + ['consts'], [PSR(k)])
            dst2 = dstT.rearrange("p c n -> p (c n)")
            if evac_eng == 'act':
                act(dst2, bk, AF.Copy, [PSR(k)], dsts)
            else:
                cp(evac_eng, dst2, bk, [PSR(k)], dsts)

        def linear1024(srcT, w, wtag, src_reg):
            k = bank2()
            bank = BK2(k)
            for h in range(2):
                for c in range(8):
                    mm(bank[:, h * 512:(h + 1) * 512], srcT[:, c, :], w[:, c, h * 512:(h + 1) * 512], c == 0, c == 7,
                       [src_reg, wtag], [PSR(k + h)])
            return bank, [PSR(k), PSR(k + 1)]

        P0 = pbs[0]
        mch = newchan()
        for mt in range(2):
            R = lambda n: ('m', n)
            dma('sp', m_f, mem_d[mt * 128:(mt + 1) * 128, :], [], [R('f')], mch)
            act(m_sq, m_f, AF.Square, [R('f')], [R('sq'), R('ss')], accum=P0.sm[:, 0:1])
            rstd_of(P0.sm[:, 1:2], P0.sm[:, 0:1], 1024, 'mr', [R('ss')], [R('rstd')], P0.sm[:, 2:3])
            ts('dve', m_b, m_f, P0.sm[:, 1:2], None, ALU.mult, None, [R('f'), R('rstd')], ['m_b'])
            transpose8(m_b, m_T, 'm_b', 'm_T', 'dve')
            bank, bregs = linear1024(m_T, w_ck, 'w_ck', 'm_T')
            act(m_sq, bank, AF.Square, bregs, [R('sq')])
            red(P0.sm[:, 4:8], m_sq.rearrange("p (h d) -> p h d", h=4), ALU.add, [R('sq')], [R('ssk')])
            rstd_of(P0.sm[:, 8:12], P0.sm[:, 4:8], 256, 'mk', [R('ssk')], [R('rk')], P0.sm[:, 12:16])
            tt('dve', m_b.rearrange("p (h d) -> p h d", h=4), bank.rearrange("p (h d) -> p h d", h=4),
               P0.sm[:, 8:12].unsqueeze(2).broadcast_to([128, 4, 256]), ALU.mult, bregs + [R('rk')], ['m_b'])
            kk = bank1()
            bk = BK(kk).bitcast(BF16)
            for c in range(8):
                tr(bk[:, c * 128:(c + 1) * 128], m_b[:, c * 128:(c + 1) * 128], ident, ['m_b', 'consts'], [PSR(kk)])
            for c in range(8):
                ts('dve', kcT[:, c, mt * 128:(mt + 1) * 128], bk[:, c * 128:(c + 1) * 128], gckq[:, (c % 2):(c % 2) + 1], None,
                   ALU.mult, None, [PSR(kk), 'consts'], ['kcT'])
            bank, bregs = linear1024(m_T, w_cv, 'w_cv', 'm_T')
            act(vc[:, mt, :], bank, AF.Copy, bregs, ['vc'])
        if phases >= 2:
            s.fence(skip_chans=(zch,), keep=xsz)
        else:
            s.fence()

        xch = [newchan() for _ in range(NSLOTS_T)]
        och = [newchan() for _ in range(NSLOTS_T)]
        sch = [newchan() for _ in range(NSLOTS_T)]
        for P in pbs:
            s.op('pool', lambda e, P=P: e.memset(P.vaug, 1.0), [], [('vaug', P.b)])

        def tile_gen(i):
            P = pbs[i % NSLOTS_T]
            Pp = pbs[(i - 1) % NSLOTS_T]
            b = P.b
            bp = Pp.b
            R = lambda n: (n, b)
            sm = P.sm
            rows = slice(i * 128, (i + 1) * 128)
            UG = [R('u'), R('g')]
            PTALL = [R(('PT', k)) for k in range(4)]
            dma('sp', P.xres, x_d[rows, :], [], [R('xres')], xch[b])
            act(P.sq, P.xres, AF.Square, [R('xres')], [R('sq'), R('ss1')], accum=sm[:, 0:1])
            rstd_of(sm[:, 1:2], sm[:, 0:1], 1024, 'r1%d' % b, [R('ss1')], [R('rstd1')], sm[:, 2:3])
            act(P.tb, P.xres, AF.Copy, [R('xres')], [R('tb'), R('tbh')])
            transpose8(P.tb, P.tT, [R('tb'), R('tbh')], R('tT'), 'dve')
            yield
            groups = [(0, 512), (512, 256), (768, 512), (1280, 512)]
            kq, kkv, ku, kg = bank1(), bank1(), bank1(), bank1()
            for kb, (c0_, w_) in zip((kq, kkv, ku, kg), groups):
                for c in range(8):
                    mm(BK(kb)[:, 0:w_], P.tT[:, c, :], w_in[:, c, c0_:c0_ + w_], c == 0, c == 7, [R('tT'), 'w_in'], [PSR(kb)])
            yield
            act(P.sq[:, 0:512], BK(kq), AF.Square, [PSR(kq), R('rstd1')], [R('sq')], scale=sm[:, 1:2])
            act(P.sq[:, 512:640], BK(kkv)[:, 0:128], AF.Square, [PSR(kkv), R('rstd1')], [R('sqk')], scale=sm[:, 1:2])
            red(sm[:, 16:26], P.sq[:, 0:640].rearrange("p (h d) -> p h d", d=64), ALU.add, [R('sq'), R('sqk')], [R('ssqk')])
            rstd_fast(sm[:, 32:42], sm[:, 16:26], 64, 'rqk%d' % b, [R('ssqk')], [R('rqk')], sm[:, 48:58])
            ts('dve', sm[:, 32:42], sm[:, 32:42], sm[:, 1:2], None, ALU.mult, None, [R('rqk'), R('rstd1')], [R('rqk')])
            tt('dve', P.qk[:, 0:512].rearrange("p (h d) -> p h d", d=64), BK(kq).rearrange("p (h d) -> p h d", d=64),
               sm[:, 32:40].unsqueeze(2).broadcast_to([128, 8, 64]), ALU.mult, [PSR(kq), R('rqk')], [R('qk')])
            tt('dve', P.qk[:, 512:640].rearrange("p (h d) -> p h d", d=64), BK(kkv)[:, 0:128].rearrange("p (h d) -> p h d", d=64),
               sm[:, 40:42].unsqueeze(2).broadcast_to([128, 2, 64]), ALU.mult, [PSR(kkv), R('rqk')], [R('qk')])
            act(P.vaug[:, :, 0:64], BK(kkv)[:, 128:256].rearrange("p (h d) -> p h d", d=64), AF.Copy, [PSR(kkv), R('rstd1')],
                [('vaug', b)], scale=sm[:, 1:2])
            act(P.ug[:, 0:512], BK(ku), AF.Gelu_apprx_tanh, [PSR(ku), R('rstd1')], [R('u')], scale=sm[:, 1:2])
            act(P.ug[:, 512:1024], BK(kg), AF.Gelu_apprx_tanh, [PSR(kg), R('rstd1')], [R('g')], scale=sm[:, 1:2])
            act(P.sq[:, 512:1024], P.ug[:, 512:1024], AF.Square, [R('g')], [R('sqk'), R('ssg')], accum=sm[:, 3:4])
            rstd_of(sm[:, 4:5], sm[:, 3:4], 512, 'rg%d' % b, [R('ssg')], [R('rg')], sm[:, 5:6])
            stt(P.gn, P.ug[:, 512:1024], sm[:, 4:5], gsgu, ALU.mult, ALU.mult, [R('g'), R('rg'), 'consts'], [R('gn')])
            yield
            kqt, kkt = bank1(), bank1()
            qTb = BK(kqt).bitcast(BF16)
            kTb = BK(kkt).bitcast(BF16)
            for h in range(8):
                tr(qTb[0:64, h * 128:(h + 1) * 128], P.qk[:, h * 64:(h + 1) * 64], ident, [R('qk'), 'consts'], [PSR(kqt)])
            for h in range(2):
                tr(kTb[0:64, h * 128:(h + 1) * 128], P.qk[:, 512 + h * 64:512 + (h + 1) * 64], ident, [R('qk'), 'consts'], [PSR(kkt)])
            cp('dve', P.qT, qTb[0:64, 0:1024], [PSR(kqt)], [R('qT')])
            ts('dve', P.kT, kTb[0:64, 0:256], gqk[0:64, 0:1], None, ALU.mult, None, [PSR(kkt), 'consts'], [('kT', b)])
            ksg = bank1()
            for g in range(8):
                mm(BK(ksg)[:, g * 64:(g + 1) * 64], wT[:, g, :], P.gn[:, g * 64:(g + 1) * 64], True, True, [R('gn'), 'wT'], [PSR(ksg)])
            yield
            order = [(0, 'prev'), (0, 'cur'), (1, 'cur'), (1, 'prev')]
            for kvh, which in order:
                if which == 'prev' and i == 0:
                    continue
                ks = bank1()
                kt = P.kT if which == 'cur' else Pp.kT
                ktreg = ('kT', b) if which == 'cur' else ('kT', bp)
                mbias = mb_cur if which == 'cur' else mb_prev
                mm(BK(ks), kt[:, kvh * 128:(kvh + 1) * 128], P.qT[:, kvh * 512:(kvh + 1) * 512], True, False,
                   [ktreg, R('qT')], [PSR(ks)])
                mm(BK(ks), ident, mbias, False, True, ['consts'], [PSR(ks)])
                pidx = kvh * 2 + (0 if which == 'prev' else 1)
                act(P.PT[:, pidx, :], BK(ks), AF.Exp, [PSR(ks)], [R(('PT', pidx))], scale=0.125)
            tt('dve', P.asg[:, 512:1024], BK(ksg), bfull, ALU.add, [PSR(ksg), 'consts'], [R('sg')])
            tt('dve', P.asg[:, 512:1024], P.asg[:, 512:1024], P.ug[:, 0:512], ALU.mult, [R('sg'), R('u')], [R('sg')])
            yield
            kov = [bank1(), bank1()]
            for kvh in range(2):
                ov = BK(kov[kvh])[:, 0:260].rearrange("p (h d) -> p h d", d=65)
                for r in range(4):
                    whs = ['cur'] if i == 0 else ['prev', 'cur']
                    for wi, which in enumerate(whs):
                        pidx = kvh * 2 + (0 if which == 'prev' else 1)
                        va = P.vaug if which == 'cur' else Pp.vaug
                        vreg = ('vaug', b) if which == 'cur' else ('vaug', bp)
                        mm(ov[:, r, :], P.PT[:, pidx, r * 128:(r + 1) * 128], va[:, kvh, 0:65], wi == 0, wi == len(whs) - 1,
                           [R(('PT', pidx)), vreg], [PSR(kov[kvh])])
            for kvh in range(2):
                ov = BK(kov[kvh])[:, 0:260].rearrange("p (h d) -> p h d", d=65)
                tt('dve', sm[:, 64 + kvh * 4:68 + kvh * 4].unsqueeze(2), ov[:, :, 64:65], esink[:, kvh * 4:kvh * 4 + 4].unsqueeze(2), ALU.add,
                   [PSR(kov[kvh]), 'consts'], [R(('den', kvh))])
                s.op('dve', lambda e, o=sm[:, 72 + kvh * 4:76 + kvh * 4], i_=sm[:, 64 + kvh * 4:68 + kvh * 4]: e.reciprocal(out=o, in_=i_),
                     [R(('den', kvh))], [R(('rden', kvh))])
                tt('dve', P.asg[:, kvh * 256:(kvh + 1) * 256].rearrange("p (h d) -> p h d", d=64), ov[:, :, 0:64],
                   sm[:, 72 + kvh * 4:76 + kvh * 4].unsqueeze(2).broadcast_to([128, 4, 64]), ALU.mult,
                   [PSR(kov[kvh]), R(('rden', kvh))], [R(('attn', kvh))])
            act(P.sq[:, 0:512], P.asg[:, 0:512], AF.Square, [R(('attn', 0)), R(('attn', 1))], [R('sq'), R('ssa')], accum=sm[:, 6:7])
            act(P.sq[:, 512:1024], P.asg[:, 512:1024], AF.Square, [R('sg')], [R('sqk'), R('sssg')], accum=sm[:, 7:8])
            rstd_fast(sm[:, 8:10], sm[:, 6:8], 512, 'ro%d' % b, [R('ssa'), R('sssg')], [R('ro')], sm[:, 10:12])
            ts('dve', P.tb[:, 0:512], P.asg[:, 0:512], sm[:, 8:9], None, ALU.mult, None, [R(('attn', 0)), R(('attn', 1)), R('ro')], [R('tb')])
            act(P.tb[:, 512:1024], P.asg[:, 512:1024], AF.Copy, [R('sg'), R('ro')], [R('tbh')], scale=sm[:, 9:10])
            yield
            transpose8(P.tb, P.tT, [R('tb'), R('tbh')], R('tT'), 'act')
            yield
            bank, bregs = linear1024(P.tT, w_out, 'w_out', R('tT'))
            tt('dve', P.xres, bank, P.xres, ALU.add, bregs + [R('xres')], [R('xres')])
            yield
            act(P.sq, P.xres, AF.Square, [R('xres')], [R('sq'), R('sqk'), R('ss2')], accum=sm[:, 12:13])
            rstd_of(sm[:, 13:14], sm[:, 12:13], 1024, 'r2%d' % b, [R('ss2')], [R('rstd2')], sm[:, 14:15])
            cp('dve', P.tb, P.xres, [R('xres')], [R('tb'), R('tbh')])
            transpose8(P.tb, P.tT, [R('tb'), R('tbh')], R('tT'), 'act')
            yield
            bank, bregs = linear1024(P.tT, w_cq, 'w_cq', R('tT'))
            yield
            act(P.sq, bank, AF.Square, bregs + [R('rstd2')], [R('sq'), R('sqk')], scale=sm[:, 13:14])
            red(sm[:, 80:84], P.sq.rearrange("p (h d) -> p h d", h=4), ALU.add, [R('sq'), R('sqk')], [R('ssqc')])
            rstd_fast(sm[:, 84:88], sm[:, 80:84], 256, 'rqc%d' % b, [R('ssqc')], [R('rqc')], sm[:, 88:92])
            ts('dve', sm[:, 84:88], sm[:, 84:88], sm[:, 13:14], None, ALU.mult, None, [R('rqc'), R('rstd2')], [R('rqc')])
            tt('dve', P.tb.rearrange("p (h d) -> p h d", h=4), bank.rearrange("p (h d) -> p h d", h=4),
               sm[:, 84:88].unsqueeze(2).broadcast_to([128, 4, 256]), ALU.mult, bregs + [R('rqc')], [R('tb'), R('tbh')])
            transpose8(P.tb, P.tT, [R('tb'), R('tbh')], R('tT'), 'act')
            yield
            ksc = bank2()
            for h in range(4):
                for mc in range(2):
                    idx = h * 2 + mc
                    for dc in range(2):
                        mm(BK2(ksc)[:, idx * 128:(idx + 1) * 128], kcT[:, h * 2 + dc, mc * 128:(mc + 1) * 128], P.tT[:, h * 2 + dc, :],
                           dc == 0, dc == 1, ['kcT', R('tT')], [PSR(ksc + idx // 4)])
            act(P.PcT.rearrange("p c n -> p (c n)"), BK2(ksc), AF.Exp, [PSR(ksc), PSR(ksc + 1)], [R('PcT')] + PTALL, scale=1.0 / 16.0)
            yield
            kpv = bank2()
            kden = bank1()
            for h in range(4):
                for mc in range(2):
                    mm(BK2(kpv)[:, h * 256:(h + 1) * 256], P.PcT[:, h * 2 + mc, :], vc[:, mc, h * 256:(h + 1) * 256], mc == 0, mc == 1,
                       [R('PcT'), 'vc'] + PTALL, [PSR(kpv + h // 2)])
            for h in range(4):
                for mc in range(2):
                    mm(BK(kden)[:, h:h + 1], P.PcT[:, h * 2 + mc, :], ones_col[:, 0:1], mc == 0, mc == 1, [R('PcT'), 'consts'] + PTALL, [PSR(kden)])
            s.op('dve', lambda e: e.reciprocal(out=sm[:, 92:96], in_=BK(kden)[:, 0:4]), [PSR(kden)], [R('rdc')])
            tt('dve', P.tb.rearrange("p (h d) -> p h d", h=4), BK2(kpv).rearrange("p (h d) -> p h d", h=4),
               sm[:, 92:96].unsqueeze(2).broadcast_to([128, 4, 256]), ALU.mult, [PSR(kpv), PSR(kpv + 1), R('rdc')], [R('tb'), R('tbh')])
            transpose8(P.tb, P.tT, [R('tb'), R('tbh')], R('tT'), 'act')
            yield
            bank, bregs = linear1024(P.tT, w_co, 'w_co', R('tT'))
            tt('dve', P.xres, bank, P.xres, ALU.add, bregs + [R('xres')], [R('xres')])
            yield
            dma('sp', out_d[rows, :], P.xres, [R('xres')], [('outd', i)], och[b])
            if phases < 2:
                yield
                return
            rt = P.rt
            act(P.sq, P.xres, AF.Square, [R('xres')], [R('sq'), R('sqk'), R('ss3')], accum=sm[:, 96:97])
            rstd_of(sm[:, 97:98], sm[:, 96:97], 1024, 'r3%d' % b, [R('ss3')], [R('rstd3')], sm[:, 98:99])
            stt(P.tb, P.xres, sm[:, 97:98], gffn_b, ALU.mult, ALU.mult, [R('xres'), R('rstd3'), 'consts'], [R('tb'), R('tbh')])
            kx = bank2()
            for c in range(8):
                tr(BK2(kx)[:, c * 128:(c + 1) * 128], P.xres[:, c * 128:(c + 1) * 128], identf, [R('xres'), 'identf'], [PSR(kx + c // 4)])
            x2Tf = P.x2T.rearrange("p c n -> p (c n)")
            act(x2Tf[:, 0:512], BK2(kx)[:, 0:512], AF.Copy, [PSR(kx)], [R('x2Ta'), R('u')])
            cp('dve', x2Tf[:, 512:1024], BK2(kx)[:, 512:1024], [PSR(kx + 1)], [R('x2Tb'), R('g')])
            yield
            kl = bank1()
            for c in range(8):
                mm(BK(kl)[:, 0:36], P.x2T[:, c, :], wr[:, c, :], c == 0, c == 7, [R('x2Ta'), R('x2Tb'), 'wr'] + UG, [PSR(kl)])
            L = rt[:, 0:36]
            ts('dve', L, BK(kl)[:, 0:36], sm[:, 97:98], None, ALU.mult, None, [PSR(kl), R('rstd3')], [R('L')])
            red(rt[:, 36:37], rt[:, 0:4], ALU.max, [R('L')], [R('mx')])
            ts('dve', rt[:, 40:44], rt[:, 0:4], rt[:, 36:37], None, ALU.is_equal, None, [R('L'), R('mx')], [R('ohg')])
            ts('dve', rt[:, 37:38], rt[:, 36:37], -1.0, None, ALU.mult, None, [R('mx')], [R('nmx')])
            act(rt[:, 44:48], rt[:, 0:4], AF.Exp, [R('L'), R('nmx')], [R('eg'), R('sumeg')], bias=rt[:, 37:38], accum=rt[:, 38:39])
            s.op('dve', lambda e: e.reciprocal(out=rt[:, 39:40], in_=rt[:, 38:39]), [R('sumeg')], [R('gw')])
            tt('dve', rt[:, 48:80].rearrange("p (g e) -> p g e", g=4), rt[:, 4:36].rearrange("p (g e) -> p g e", g=4),
               rt[:, 40:44].unsqueeze(2).broadcast_to([128, 4, 8]), ALU.mult, [R('L'), R('ohg')], [R('ml')])
            red(rt[:, 80:88], rt[:, 48:80].rearrange("p (g e) -> p e g", g=4), ALU.add, [R('ml')], [R('loc')])
            s.op('dve', lambda e: e.max(out=rt[:, 88:96], in_=rt[:, 80:88]), [R('loc')], [R('m8')])
            ts('dve', rt[:, 96:104], rt[:, 80:88], rt[:, 88:89], None, ALU.is_equal, None, [R('loc'), R('m8')], [R('oh1')])
            ts('dve', rt[:, 104:112], rt[:, 80:88], rt[:, 89:90], None, ALU.is_equal, None, [R('loc'), R('m8')], [R('oh2')])
            tt('dve', rt[:, 112:113], rt[:, 89:90], rt[:, 88:89], ALU.subtract, [R('m8')], [R('dlt')])
            act(rt[:, 113:114], rt[:, 112:113], AF.Exp, [R('dlt')], [R('e2')])
            ts('dve', rt[:, 114:115], rt[:, 113:114], 1.0, None, ALU.add, None, [R('e2')], [R('den2')])
            s.op('dve', lambda e: e.reciprocal(out=rt[:, 115:116], in_=rt[:, 114:115]), [R('den2')], [R('p1')])
            tt('dve', rt[:, 116:117], rt[:, 115:116], rt[:, 39:40], ALU.mult, [R('p1'), R('gw')], [R('g1')])
            tt('dve', rt[:, 117:118], rt[:, 116:117], rt[:, 113:114], ALU.mult, [R('g1'), R('e2')], [R('g2')])
            OH1 = rt[:, 128:160]
            OH2 = rt[:, 160:192]
            tt('dve', OH1.rearrange("p (g e) -> p g e", g=4), rt[:, 40:44].unsqueeze(2).broadcast_to([128, 4, 8]),
               rt[:, 96:104].unsqueeze(1).broadcast_to([128, 4, 8]), ALU.mult, [R('ohg'), R('oh1')], [R('OH1')])
            tt('dve', OH2.rearrange("p (g e) -> p g e", g=4), rt[:, 40:44].unsqueeze(2).broadcast_to([128, 4, 8]),
               rt[:, 104:112].unsqueeze(1).broadcast_to([128, 4, 8]), ALU.mult, [R('ohg'), R('oh2')], [R('OH2')])
            tt('dve', P.Mb, OH1, OH2, ALU.add, [R('OH1'), R('OH2')], [R('Mb')])
            yield
            kc = bank1()
            mm(BK(kc)[:, 0:32], ustrict, P.Mb, True, True, [R('Mb'), 'consts'], [PSR(kc)])
            mm(BK(kc)[:, 32:64], ones_bf, P.Mb, True, True, [R('Mb'), 'consts'], [PSR(kc)])
            posv = rt[:, 192:224]
            tt('dve', posv, BK(kc)[:, 0:32], cnt, ALU.add, [PSR(kc), 'cnt'], [R('pos')])
            tt('dve', cnt, BK(kc)[:, 32:64], cnt, ALU.add, [PSR(kc), 'cnt'], ['cnt'])
            ts('dve', rt[:, 224:256], posv, float(CAP), 1.0e6, ALU.is_ge, ALU.mult, [R('pos')], [R('ovf')])
            tt('dve', posv, posv, rt[:, 224:256], ALU.add, [R('pos'), R('ovf')], [R('pos')])
            tt('dve', posv, posv, eoff, ALU.add, [R('pos'), 'consts'], [R('pos')])
            tt('dve', rt[:, 256:288], posv, OH1, ALU.mult, [R('pos'), R('OH1')], [R('t1')])
            red(rt[:, 118:119], rt[:, 256:288], ALU.add, [R('t1')], [R('sl1')])
            tt('dve', rt[:, 288:320], posv, OH2, ALU.mult, [R('pos'), R('OH2')], [R('t2')])
            red(rt[:, 119:120], rt[:, 288:320], ALU.add, [R('t2')], [R('sl2')])
            cp('dve', slot_i[:, i, :], rt[:, 118:120], [R('sl1'), R('sl2')], [('slot', i)])
            ts('dve', rt[:, 120:122], rt[:, 118:120], float(NSLOT), None, ALU.is_lt, None, [R('sl1'), R('sl2')], [R('ok')])
            tt('dve', gate_f[:, i, :], rt[:, 116:118], rt[:, 120:122], ALU.mult, [R('g1'), R('g2'), R('ok')], [('gate', i)])
            for k in range(2):
                s.op('pool', lambda e, k=k: e.indirect_dma_start(
                    out=xs_d[:, :], out_offset=bass.IndirectOffsetOnAxis(ap=slot_i[:, i, k:k + 1], axis=0),
                    in_=P.tb[:, :], in_offset=None, bounds_check=bcreg(e), oob_is_err=False),
                    [R('tb'), R('tbh'), ('slot', i)] + xsz, [('xs', i, k)], chan=sch[b])
            yield

        gens = [tile_gen(i) for i in range(NT)]
        live = []
        nxt = 0
        NSTG = 18
        step = 0
        while nxt < NT or live:
            if nxt < NT and len(live) < NSLOTS_T and (not live or step % (NSTG // NSLOTS_T) == 0):
                live.append(gens[nxt])
                nxt += 1
            for g in list(live):
                try:
                    next(g)
                except StopIteration:
                    live.remove(g)
            step += 1

        if phases >= 2:
            s.fence()
            s.reschedule(1)
            ar.reset(0)
            w1b = [ar.alloc([128, 8, 512], BF16) for _ in range(3)]
            w3b = [ar.alloc([128, 8, 512], BF16) for _ in range(3)]
            w2b = [ar.alloc([128, 4, 1024], BF16) for _ in range(3)]
            xst = [ar.alloc([128, 4, 1024], BF16) for _ in range(2)]
            xsT = [ar.alloc([128, 8, 512], BF16) for _ in range(2)]
            hidT = [ar.alloc([128, 4, 512], BF16) for _ in range(2)]
            slt = [ar.alloc([128, 512], F32) for _ in range(2)]
            yo = [ar.alloc([128, 1024], BF16) for _ in range(3)]
            p3buf = ar.off
            wch = [[newchan() for _ in range(3)] for _ in range(3)]
            xsch = [newchan() for _ in range(2)]
            ych = [newchan() for _ in range(3)]
            NB = CAP // 128
            NE = NEXP if phases >= 2 else 0

            def load_weights(e):
                wb = e % 3
                dma('pool', w1b[wb], dr["w1"][e].rearrange("(c p) f -> p c f", p=128), [], [('w1', wb, 0)], wch[wb][0])
                dma('pool', w3b[wb], dr["w3"][e].rearrange("(c p) f -> p c f", p=128), [], [('w3', wb, 0)], wch[wb][1])
                dma('pool', w2b[wb], dr["w2"][e].rearrange("(c p) f -> p c f", p=128), [], [('w2', wb, 0)], wch[wb][2])

            def load_xs(e):
                eb = e % 2
                dma('sp', xst[eb], xs_d[e * CAP:(e + 1) * CAP, :].rearrange("(sb p) d -> p sb d", p=128), [], [('xst', eb)], xsch[eb])

            def do_T(e):
                eb = e % 2
                for sb in range(NB):
                    bank = pp[0][:, (sb % 2) * 512:(sb % 2) * 512 + 512].bitcast(BF16)
                    breg = ('p2', sb % 2)
                    for c in range(8):
                        tr(bank[:, c * 128:(c + 1) * 128], xst[eb][:, sb, c * 128:(c + 1) * 128], ident, [('xst', eb), 'consts'], [breg])
                    dst = xsT[eb][:, :, sb * 128:(sb + 1) * 128]
                    src = bank.rearrange("p (c n) -> p c n", c=8)
                    if sb % 2 == 0:
                        act(dst, src, AF.Copy, [breg], [('xsT', eb, sb)])
                    else:
                        cp('dve', dst, src, [breg], [('xsT', eb, sb)])

            def do_H(e):
                eb = e % 2
                wb = e % 3
                xreg = [('xsT', eb, sb) for sb in range(NB)]
                for fc in range(4):
                    hb = pp[1 + fc % 2]
                    for c in range(8):
                        mm(hb[:, 0:512], w1b[wb][:, c, fc * 128:(fc + 1) * 128], xsT[eb][:, c, :], c == 0, c == 7,
                           xreg + [('w1', wb, 0)], [('p2h', fc % 2, 0)])
                    for c in range(8):
                        mm(hb[:, 512:1024], w3b[wb][:, c, fc * 128:(fc + 1) * 128], xsT[eb][:, c, :], c == 0, c == 7,
                           xreg + [('w3', wb, 0)], [('p2h', fc % 2, 1)])
                    act(slt[fc % 2], hb[:, 0:512], AF.Silu, [('p2h', fc % 2, 0)], [('slt', fc % 2)])
                    tt('dve', hidT[eb][:, fc, :], hb[:, 512:1024], slt[fc % 2], ALU.mult, [('p2h', fc % 2, 1), ('slt', fc % 2)], [('hidT', eb, fc)])

            ycount = [0]

            def do_Y(e):
                eb = e % 2
                wb = e % 3
                hreg = [('hidT', eb, fc) for fc in range(4)]
                for sb in range(NB):
                    ob = ycount[0] % 3
                    ycount[0] += 1
                    for hlf in range(2):
                        bank = pp[3][:, hlf * 512:(hlf + 1) * 512]
                        for fc in range(4):
                            mm(bank, hidT[eb][:, fc, sb * 128:(sb + 1) * 128], w2b[wb][:, fc, hlf * 512:(hlf + 1) * 512], fc == 0, fc == 3,
                               hreg + [('w2', wb, 0)], [('p2y', hlf)])
                        if hlf == 0:
                            act(yo[ob][:, 0:512], bank, AF.Copy, [('p2y', 0)], [('yo', ob, 0)])
                        else:
                            cp('dve', yo[ob][:, 512:1024], bank, [('p2y', 1)], [('yo', ob, 1)])
                    r0 = e * CAP + sb * 128
                    dma('sp', ys_d[r0:r0 + 128, :], yo[ob], [('yo', ob, 0), ('yo', ob, 1)], [('ys', e, sb)], ych[ob])

            load_weights(0)
            load_weights(1)
            load_xs(0)
            do_T(0)
            for e in range(NE):
                if e + 2 < NE:
                    load_weights(e + 2)
                if e + 1 < NE:
                    load_xs(e + 1)
                do_H(e)
                if e + 1 < NE:
                    do_T(e + 1)
                do_Y(e)
            s.fence()
            ar.reset(p3buf)
            cb = []
            NB3 = 4
            for b in range(NB3):
                cb.append((ar.alloc([128, 1024], F32), ar.alloc([128, 1024], BF16), ar.alloc([128, 1024], BF16)))
            gch = [[newchan(), newchan()] for _ in range(NB3)]
            lch = [newchan() for _ in range(NB3)]
            fch = [newchan() for _ in range(NB3)]
            for b in range(NB3):
                s.op('dve', lambda e, b=b: e.memset(cb[b][1], 0.0), [], [('y1', b)])
                s.op('pool', lambda e, b=b: e.memset(cb[b][2], 0.0), [], [('y2', b)])
            for i in range(NT):
                b = i % NB3
                xr, y1, y2 = cb[b]
                rows = slice(i * 128, (i + 1) * 128)
                dma('sp', xr, out_d[rows, :], [], [('xr', b)], lch[b])
                for k, yb in ((0, y1), (1, y2)):
                    s.op('pool', lambda e, k=k, yb=yb, i=i: e.indirect_dma_start(
                        out=yb[:, :], out_offset=None, in_=ys_d[:, :],
                        in_offset=bass.IndirectOffsetOnAxis(ap=slot_i[:, i, k:k + 1], axis=0),
                        bounds_check=bcreg(e), oob_is_err=False), [], [('y%d' % (k + 1), b)], chan=gch[b][k])
                stt(xr, y1, gate_f[:, i, 0:1], xr, ALU.mult, ALU.add, [('xr', b), ('y1', b)], [('xr', b)])
                stt(xr, y2, gate_f[:, i, 1:2], xr, ALU.mult, ALU.add, [('xr', b), ('y2', b)], [('xr', b)])
                dma('sp', out_d[rows, :], xr, [('xr', b)], [('outd', i)], fch[b])
        s.op('sp', lambda e: e.nop(), [('outd', i) for i in range(NT)], [])
        s.emit(block)
        stats = {e: len(s.ops[e]) for e in ENGS}
        stats['waits'] = s.nwaits
        nc._kstats = stats
    return nc


_W_NAMES = ["norm_mix", "q_norm", "k_norm", "attn_sinks", "sgu_norm", "out_norm_attn", "out_norm_sgu", "norm_cross",
            "norm_mem", "cq_norm", "ck_norm", "norm_ffn", "w_in", "w_out", "w_cq", "w_ck", "w_cv", "w_co", "sgu_w",
            "sgu_b", "w_router_group", "w_router_expert", "w1", "w3", "w2"]


def kernel(**inputs):
    x = np.ascontiguousarray(inputs["x"], dtype=np.float32)
    mem = np.ascontiguousarray(inputs["mem"], dtype=np.float32)
    shared = {k: np.ascontiguousarray(inputs[k], dtype=np.float32) for k in _W_NAMES}
    nc = build(NT=32, phases=3)
    in_maps = []
    for c in range(NCORES):
        m = dict(shared)
        m["x"] = x[c]
        m["mem"] = mem[c]
        in_maps.append(m)
    res = run_bass_kernel_spmd(nc, in_maps, core_ids=list(range(NCORES)))
    return np.stack([r["out"] for r in res.results], axis=0).astype(np.float32)
```

```python
import numpy as np
from contextlib import ExitStack
import concourse.bass as bass
import concourse.mybir as mybir
from concourse.bass_utils import run_bass_kernel_spmd

F32 = mybir.dt.float32
BF16 = mybir.dt.bfloat16
I32 = mybir.dt.int32
U8 = mybir.dt.uint8
AF = mybir.ActivationFunctionType
ALU = mybir.AluOpType
AX = mybir.AxisListType

ENGS = ['pe', 'act', 'dve', 'pool', 'sp']
EPS = 1e-6
D = 1024
SEQ = 4096
NCORES = 8
NEXP = 32
CAP = 512
NEG = -30000.0
PSUM_TAGS = ('ps', 'p2', 'p2h', 'p2y')


class Chan:
    def __init__(self, sem):
        self.sem = sem
        self.count = 0


class _Op:
    __slots__ = ('eng', 'fn', 'deps', 'odeps', 'signal', 'chan', 'val', 'dur', 'seg', 'cval')

    def __init__(self):
        self.signal = False
        self.chan = None
        self.val = None


class Sched:
    def __init__(self, csem, chans):
        self.csem = csem
        self.chans = chans
        self.ops = {e: [] for e in ENGS}
        self.last_w = {}
        self.readers = {}
        self.nwaits = 0
        self.seg = 0
        self.dma_op = {}

    def op(self, eng, fn, reads=(), writes=(), chan=None, relax_same=False, dur=None):
        o = _Op()
        o.eng = eng
        o.fn = fn
        o.dur = dur if dur is not None else (2500.0 if chan is not None else 300.0)
        o.seg = self.seg
        o.odeps = []
        deps = set()
        for r in reads:
            t = self.last_w.get(r)
            if t is not None:
                deps.add(t)
            if isinstance(r, tuple) and r[0] in PSUM_TAGS:
                for t in self.readers.get(r, ()):
                    if not (t[0] == 'c' and t[1] == eng):
                        deps.add(t)
        for w in writes:
            t = self.last_w.get(w)
            if t is not None:
                deps.add(t)
            for t in self.readers.get(w, ()):
                deps.add(t)
        if chan is not None:
            chan.count += 16
            o.chan = chan
            o.cval = chan.count
            ticket = ('d', chan, chan.count)
            self.dma_op[(id(chan), chan.count)] = (eng, len(self.ops[eng]))
        else:
            ticket = ('c', eng, len(self.ops[eng]))
        final = []
        for d in deps:
            if d[0] == 'c':
                if d[1] == eng and chan is None and (eng == 'pe' or relax_same):
                    o.odeps.append(d)
                    continue
                self.ops[d[1]][d[2]].signal = True
            final.append(d)
        o.deps = final
        key = (ticket[0], ticket[1])
        for r in reads:
            lst = self.readers.setdefault(r, [])
            for n, t in enumerate(lst):
                if (t[0], t[1]) == key:
                    lst[n] = ticket
                    break
            else:
                lst.append(ticket)
        for w in writes:
            self.last_w[w] = ticket
            self.readers[w] = []
        self.ops[eng].append(o)
        return ticket

    def fence(self, skip_chans=(), keep=()):
        kept = {r: self.last_w[r] for r in keep if r in self.last_w}
        tickets = []
        for e in ENGS:
            idx = None
            for i in range(len(self.ops[e]) - 1, -1, -1):
                if self.ops[e][i].chan is None and self.ops[e][i].fn is not None:
                    idx = i
                    break
            if idx is not None:
                self.ops[e][idx].signal = True
                tickets.append(('c', e, idx))
        for c in self.chans:
            if c.count and c not in skip_chans:
                tickets.append(('d', c, c.count))
        for e in ENGS:
            o = _Op()
            o.eng = e
            o.fn = None
            o.deps = list(tickets)
            o.odeps = []
            o.dur = 0.0
            o.seg = self.seg
            self.ops[e].append(o)
        self.seg += 1
        self.last_w = dict(kept)
        self.readers = {}

    def reschedule(self, seg, window=64, hop=300.0):
        rng = {}
        for e in ENGS:
            idx = [i for i, o in enumerate(self.ops[e]) if o.seg == seg and o.fn is not None]
            if idx:
                assert idx == list(range(idx[0], idx[-1] + 1))
                rng[e] = (idx[0], idx[-1] + 1)
        done = {}
        pend = {e: list(range(*rng[e])) for e in rng}
        free = {e: 0.0 for e in rng}
        new_order = {e: [] for e in rng}

        def prod(d):
            if d[0] == 'c':
                return (d[1], d[2])
            return self.dma_op[(id(d[1]), d[2])]

        def start_of(e, i):
            o = self.ops[e][i]
            t = free[e]
            for d in o.deps:
                pe_, pi = prod(d)
                if pe_ in rng and rng[pe_][0] <= pi < rng[pe_][1]:
                    c = done.get((pe_, pi))
                    if c is None:
                        return None
                    t = max(t, c + (0.0 if pe_ == e else hop))
            for d in o.odeps:
                pe_, pi = d[1], d[2]
                if pe_ in rng and rng[pe_][0] <= pi < rng[pe_][1]:
                    c = done.get((pe_, pi))
                    if c is None:
                        return None
                    t = max(t, c)
            return t

        total = sum(len(v) for v in pend.values())
        for _ in range(total):
            best = None
            for e in pend:
                lst = pend[e]
                if not lst:
                    continue
                w = 1 if e in ('sp', 'pool') else window
                for pos in range(min(w, len(lst))):
                    st = start_of(e, lst[pos])
                    if st is None:
                        continue
                    key = (st, pos)
                    if best is None or key < best[0]:
                        best = (key, e, pos)
                    if st <= free[e]:
                        break
            assert best is not None, "scheduler deadlock"
            (st, _), e, pos = best
            i = pend[e].pop(pos)
            o = self.ops[e][i]
            done[(e, i)] = st + o.dur
            free[e] = st + (o.dur if o.chan is None else 60.0)
            new_order[e].append(i)
        remap = {}
        for e in rng:
            lo = rng[e][0]
            for newpos, i in enumerate(new_order[e]):
                remap[(e, i)] = lo + newpos
        for e in rng:
            lo, hi = rng[e]
            self.ops[e][lo:hi] = [self.ops[e][i] for i in new_order[e]]
        for key in list(self.dma_op.keys()):
            v = self.dma_op[key]
            if v in remap:
                self.dma_op[key] = (v[0], remap[v])

        def fix(d):
            if d[0] == 'c' and (d[1], d[2]) in remap:
                return ('c', d[1], remap[(d[1], d[2])])
            return d
        for e in ENGS:
            for o in self.ops[e]:
                o.deps = [fix(d) for d in o.deps]
                o.odeps = [fix(d) for d in o.odeps]
        self.sim_span = max(done.values()) if done else 0.0
        for e in ENGS:
            if e not in rng:
                continue
            lo, hi = rng[e]
            if hi < len(self.ops[e]) and self.ops[e][hi].fn is None:
                last = None
                for i in range(hi - 1, lo - 1, -1):
                    if self.ops[e][i].chan is None:
                        last = i
                        break
                for e2 in ENGS:
                    f = self.ops[e2][rng[e2][1]] if e2 in rng else None
                    if f is None or f.fn is not None:
                        continue
                    f.deps = [d for d in f.deps if not (d[0] == 'c' and d[1] == e and lo <= d[2] < hi)]
                    if last is not None:
                        f.deps.append(('c', e, last))
                if last is not None:
                    self.ops[e][last].signal = True

    def emit(self, block):
        for e in ENGS:
            c = 0
            for o in self.ops[e]:
                if o.signal:
                    c += 1
                    o.val = c

        def run(e, eng):
            waited = {}
            for o in self.ops[e]:
                need = {}
                for d in o.deps:
                    if d[0] == 'c':
                        sem = self.csem[d[1]]
                        val = self.ops[d[1]][d[2]].val
                    else:
                        sem = d[1].sem
                        val = d[2]
                    k = id(sem)
                    if k not in need or need[k][1] < val:
                        need[k] = (sem, val)
                for k, (sem, val) in need.items():
                    if waited.get(k, 0) >= val:
                        continue
                    eng.wait_ge(sem, val)
                    self.nwaits += 1
                    waited[k] = val
                if o.fn is None:
                    continue
                ins = o.fn(eng)
                if o.chan is not None:
                    ins.then_inc(o.chan.sem, 16)
                elif o.signal:
                    ins.then_inc(self.csem[e], 1)

        @block.tensor
        def _(eng):
            run('pe', eng)

        @block.scalar
        def _(eng):
            run('act', eng)

        @block.vector
        def _(eng):
            run('dve', eng)

        @block.gpsimd
        def _(eng):
            run('pool', eng)

        @block.sync
        def _(eng):
            run('sp', eng)


class Arena:
    def __init__(self, t, nbytes):
        self.t = t
        self.n = nbytes
        self.off = 0

    def reset(self, off=0):
        self.off = off

    def alloc(self, shape, dt):
        esz = 4 if dt in (F32, I32) else 2
        per = esz
        for d in shape[1:]:
            per *= d
        self.off = (self.off + 63) // 64 * 64
        assert self.off + per <= self.n, ("arena overflow", self.off, per, self.n)
        v = self.t[0:shape[0], self.off:self.off + per].bitcast(dt)
        self.off += per
        if len(shape) == 3:
            v = v.rearrange("p (a b) -> p a b", a=shape[1])
        elif len(shape) == 4:
            v = v.rearrange("p (a b c) -> p a b c", a=shape[1], b=shape[2])
        return v


def build(NT=32, phases=3):
    S = NT * 128
    nc = bass.Bass("TRN2", target_bir_lowering=False)
    dr = {}

    def din(name, shape, dt=F32):
        dr[name] = nc.dram_tensor(name, list(shape), dt, kind="ExternalInput").ap()
        return dr[name]

    x_d = din("x", [S, D])
    mem_d = din("mem", [256, D])
    for nm, n in [("norm_mix", 1024), ("q_norm", 64), ("k_norm", 64), ("attn_sinks", 8), ("sgu_norm", 512),
                  ("out_norm_attn", 512), ("out_norm_sgu", 512), ("norm_cross", 1024), ("norm_mem", 1024),
                  ("cq_norm", 256), ("ck_norm", 256), ("norm_ffn", 1024)]:
        din(nm, [n])
    din("w_in", [D, 1792]); din("w_out", [D, D]); din("w_cq", [D, D]); din("w_ck", [D, D])
    din("w_cv", [D, D]); din("w_co", [D, D])
    din("sgu_w", [8, 128, 128]); din("sgu_b", [8, 128])
    din("w_router_group", [D, 4]); din("w_router_expert", [4, D, 8])
    din("w1", [NEXP, D, 512]); din("w3", [NEXP, D, 512]); din("w2", [NEXP, 512, D])
    out_d = nc.dram_tensor("out", [S, D], F32, kind="ExternalOutput").ap()
    NSLOT = NEXP * CAP
    xs_d = nc.dram_tensor("xs_scr", [NSLOT, D], BF16, kind="Internal").ap()
    ys_d = nc.dram_tensor("ys_scr", [NSLOT, D], BF16, kind="Internal").ap()

    with ExitStack() as es:
        E = es.enter_context
        arena_t = E(nc.sbuf_tensor("arena", [128, 186 * 1024], U8))
        pers_t = E(nc.sbuf_tensor("pers", [128, 16 * 1024], U8))
        pp = [E(nc.psum_tensor("pp%d" % i, [128, 1024], F32)) for i in range(4)]
        csem = {e: E(nc.semaphore("c_" + e)) for e in ENGS}
        chans = [Chan(E(nc.semaphore("dma%d" % i))) for i in range(80)]
        block = E(nc.Block())
        s = Sched(csem, chans)
        ar = Arena(arena_t, 186 * 1024)
        pa = Arena(pers_t, 16 * 1024)
        chi = [0]

        regs = {}

        def bcreg(e):
            if 'bc' not in regs:
                regs['bc'] = e.to_reg(NSLOT - 1)
            return regs['bc']

        def newchan():
            c = chans[chi[0]]
            chi[0] += 1
            return c

        def nfree(ap):
            n = 1
            for d in ap.shape[1:]:
                n *= d
            return n

        def edur(eng, ap, extra=0.0):
            n = nfree(ap)
            if eng == 'act':
                return 220.0 + 1.04 * n + extra
            if eng == 'dve':
                return 120.0 + 1.04 * n + extra
            return 600.0 + 2.2 * n + extra

        def dma(eng, out, in_, reads, writes, chan, **kw):
            s.op(eng, lambda e: e.dma_start(out=out, in_=in_, **kw), reads, writes, chan=chan)

        def mm(out, lhsT, rhs, start, stop, reads, writes):
            s.op('pe', lambda e: e.matmul(out, lhsT=lhsT, rhs=rhs, start=start, stop=stop), reads, writes,
                 dur=20.0 + 0.8 * max(nfree(rhs), 64) * (4 if rhs.dtype == F32 else 1))

        def tr(out, in_, idt, reads, writes):
            s.op('pe', lambda e: e.transpose(out=out, in_=in_, identity=idt), reads, writes, dur=20.0 + 0.8 * 128)

        def act(out, in_, func, reads, writes, scale=None, bias=None, accum=None):
            kw = {}
            if scale is not None:
                kw['scale'] = scale
            if bias is not None:
                kw['bias'] = bias
            if accum is not None:
                kw['accum_out'] = accum
            s.op('act', lambda e: e.activation(out=out, in_=in_, func=func, **kw), reads, writes,
                 dur=edur('act', out, 100.0 if accum is not None else 0.0))

        def tt(eng, out, in0, in1, op, reads, writes):
            s.op(eng, lambda e: e.tensor_tensor(out=out, in0=in0, in1=in1, op=op), reads, writes, dur=edur(eng, out))

        def ts(eng, out, in0, s1, s2, op0, op1, reads, writes):
            if op1 is None:
                s.op(eng, lambda e: e.tensor_scalar(out=out, in0=in0, scalar1=s1, scalar2=None, op0=op0), reads, writes, dur=edur(eng, out))
            else:
                s.op(eng, lambda e: e.tensor_scalar(out=out, in0=in0, scalar1=s1, scalar2=s2, op0=op0, op1=op1), reads, writes, dur=edur(eng, out))

        def stt(out, in0, scalar, in1, op0, op1, reads, writes):
            s.op('dve', lambda e: e.scalar_tensor_tensor(out=out, in0=in0, scalar=scalar, in1=in1, op0=op0, op1=op1), reads, writes,
                 dur=edur('dve', out))

        def cp(eng, out, in_, reads, writes):
            s.op(eng, lambda e: e.tensor_copy(out=out, in_=in_), reads, writes, dur=edur(eng, out))

        def red(out, in_, op, reads, writes):
            s.op('dve', lambda e: e.tensor_reduce(out=out, in_=in_, axis=AX.X, op=op), reads, writes, dur=edur('dve', in_))

        def rstd_of(out, ss, n, tag, reads, writes, tmp):
            ts('pool', tmp, ss, 1.0 / n, EPS, ALU.mult, ALU.add, reads, [tag + '_t'])
            w = neghalf[0:out.shape[0], 0:out.shape[1]]
            tt('pool', out, tmp, w, ALU.pow, [tag + '_t', 'consts'], writes)

        def rstd_fast(out, ss, n, tag, reads, writes, tmp):
            act(tmp, ss, AF.Ln, reads + ['consts'], [tag + '_t'], scale=1.0 / n, bias=epsc[0:out.shape[0], 0:1])
            act(out, tmp, AF.Exp, [tag + '_t'], writes, scale=-0.5)

        ident = pa.alloc([128, 128], BF16)
        identf = pa.alloc([128, 128], F32)
        ones_bf = pa.alloc([128, 128], BF16)
        ustrict = pa.alloc([128, 128], BF16)
        mb_cur = pa.alloc([128, 512], BF16)
        mb_prev = pa.alloc([128, 512], BF16)
        neghalf = pa.alloc([128, 16], F32)
        epsc = pa.alloc([128, 2], F32)
        gsgu = pa.alloc([128, 512], F32)
        bfull = pa.alloc([128, 512], F32)
        esink = pa.alloc([128, 8], F32)
        gmix = pa.alloc([128, 8], F32)
        gout = pa.alloc([128, 8], F32)
        gcross = pa.alloc([128, 8], F32)
        gmem = pa.alloc([128, 8], F32)
        gffn = pa.alloc([128, 8], F32)
        gqk = pa.alloc([128, 2], F32)
        gckq = pa.alloc([128, 4], F32)
        btmp = pa.alloc([128, 8], F32)
        ones_col = pa.alloc([128, 2], BF16)
        slot_i = pa.alloc([128, NT, 2], I32)
        gate_f = pa.alloc([128, NT, 2], F32)
        cnt = pa.alloc([128, 32], F32)
        eoff = pa.alloc([128, 32], F32)
        small = pa.alloc([128, 384], F32)
        gffn_b = pa.alloc([128, 1024], F32)

        c0 = newchan()
        ld = []

        cparts = []

        def cload(out, in_, **kw):
            cparts.append(('cpart', len(cparts)))
            dma('sp', out, in_, [], [cparts[-1]], c0, **kw)

        s.op('pool', lambda e: e.memset(identf, 0.0), [], ['identf'])
        s.op('pool', lambda e: e.affine_select(out=identf, in_=identf, pattern=[[-1, 128]], compare_op=ALU.not_equal,
                                               fill=1.0, base=0, channel_multiplier=1), ['identf'], ['identf'])
        cp('dve', ident, identf, ['identf'], ['consts'])
        s.op('dve', lambda e: e.memset(ones_bf, 1.0), [], ['consts'])
        s.op('dve', lambda e: e.memset(ones_col, 1.0), [], ['consts'])
        s.op('dve', lambda e: e.memset(epsc, EPS), [], ['consts'])
        s.op('dve', lambda e: e.memset(cnt, 0.0), [], ['cnt'])
        ztmp = ar.alloc([128, 512], F32)
        otmp = ar.alloc([128, 128], F32)
        s.op('pool', lambda e: e.memset(ztmp, 0.0), [], ['ztmp'])
        s.op('pool', lambda e: e.memset(otmp, 1.0), [], ['otmp'])
        s.op('pool', lambda e: e.affine_select(out=mb_cur, in_=ztmp, pattern=[[0, 4], [1, 128]], compare_op=ALU.is_ge,
                                               fill=NEG, base=0, channel_multiplier=-1), ['ztmp'], ['consts'])
        s.op('pool', lambda e: e.affine_select(out=mb_prev, in_=ztmp, pattern=[[0, 4], [-1, 128]], compare_op=ALU.is_ge,
                                               fill=NEG, base=-1, channel_multiplier=1), ['ztmp'], ['consts'])
        s.op('pool', lambda e: e.affine_select(out=ustrict, in_=otmp, pattern=[[1, 128]], compare_op=ALU.is_ge,
                                               fill=0.0, base=-1, channel_multiplier=-1), ['otmp'], ['consts'])
        s.op('pool', lambda e: e.iota(eoff, pattern=[[CAP, 32]], base=0, channel_multiplier=0,
                                      allow_small_or_imprecise_dtypes=True), [], ['consts'])

        def pc(v, c=128):
            return v.rearrange("(c p) -> p c", p=c)

        nsc = dict(allow_slow_non_contiguous=True)
        cload(gmix, pc(dr["norm_mix"]), **nsc)
        cload(gout[:, 0:4], pc(dr["out_norm_attn"]), **nsc)
        cload(gout[:, 4:8], pc(dr["out_norm_sgu"]), **nsc)
        cload(gcross, pc(dr["norm_cross"]), **nsc)
        cload(gmem, pc(dr["norm_mem"]), **nsc)
        cload(gffn, pc(dr["norm_ffn"]), **nsc)
        cload(gqk[0:64, 0:1], dr["q_norm"].rearrange("(p o) -> p o", o=1))
        cload(gqk[0:64, 1:2], dr["k_norm"].rearrange("(p o) -> p o", o=1))
        cload(gckq[:, 0:2], pc(dr["cq_norm"]), **nsc)
        cload(gckq[:, 2:4], pc(dr["ck_norm"]), **nsc)
        cload(gsgu, dr["sgu_norm"].partition_broadcast(128))
        cload(esink, dr["attn_sinks"].partition_broadcast(128))
        cload(btmp, dr["sgu_b"].rearrange("g i -> i g"), **nsc)
        cload(gffn_b, dr["norm_ffn"].partition_broadcast(128))
        s.op('dve', lambda e: e.memset(neghalf, -0.5), cparts, ['consts'])
        tt('dve', gqk[0:64, 0:1], gqk[0:64, 0:1], gqk[0:64, 1:2], ALU.mult, ['consts'], ['consts'])
        tt('dve', gckq[:, 0:2], gckq[:, 0:2], gckq[:, 2:4], ALU.mult, ['consts'], ['consts'])
        act(esink, esink, AF.Exp, ['consts'], ['consts'])
        cp('dve', bfull.rearrange("p (g d) -> p g d", d=64), btmp.unsqueeze(2).broadcast_to([128, 8, 64]), ['consts'], ['consts'])

        NSLOTS_T = 3
        ar.reset(0)
        w_in = ar.alloc([128, 8, 1792], BF16)
        w_out = ar.alloc([128, 8, 1024], BF16)
        w_cq = ar.alloc([128, 8, 1024], BF16)
        w_co = ar.alloc([128, 8, 1024], BF16)
        kcT = ar.alloc([128, 8, 256], BF16)
        vc = ar.alloc([128, 2, 1024], BF16)
        wT = ar.alloc([128, 8, 128], BF16)
        wr = ar.alloc([128, 8, 36], F32)
        zt = ar.alloc([128, 1024], BF16)

        class PB:
            pass
        pbs = []
        slot_off = []
        for b in range(NSLOTS_T):
            slot_off.append(ar.off)
            P = PB()
            P.xres = ar.alloc([128, 1024], F32)
            P.tb = ar.alloc([128, 1024], BF16)
            P.tT = ar.alloc([128, 8, 128], BF16)
            P.ug = ar.alloc([128, 1024], F32)
            P.x2T = P.ug.rearrange("p (c n) -> p c n", c=8)
            P.sq = ar.alloc([128, 1024], F32)
            P.asg = ar.alloc([128, 1024], F32)
            P.PT = ar.alloc([128, 4, 512], BF16)
            P.PcT = P.PT.rearrange("p a b -> p (a b)")[:, 0:1024].rearrange("p (c n) -> p c n", c=8)
            P.qk = ar.alloc([128, 640], BF16)
            P.qT = ar.alloc([64, 1024], BF16)
            P.kT = ar.alloc([64, 256], BF16)
            P.vaug = ar.alloc([128, 2, 66], BF16)
            P.gn = ar.alloc([128, 512], BF16)
            P.rt = ar.alloc([128, 320], F32)
            P.Mb = ar.alloc([128, 32], BF16)
            P.sm = small[:, b * 128:(b + 1) * 128]
            P.b = b
            pbs.append(P)
        ar_end1 = ar.off
        ar.reset(slot_off[0])
        w_ck = ar.alloc([128, 8, 1024], BF16)
        w_cv = ar.alloc([128, 8, 1024], BF16)
        NSTG_W = 4
        stage = [ar.alloc([128, 1792], F32) for _ in range(NSTG_W)]
        m_f = ar.alloc([128, 1024], F32)
        m_b = ar.alloc([128, 1024], BF16)
        m_T = ar.alloc([128, 8, 128], BF16)
        m_sq = ar.alloc([128, 1024], F32)
        assert ar.off <= ar_end1, (ar.off, ar_end1)

        bptr = [0]

        def bank1():
            k = bptr[0] % 8
            bptr[0] += 1
            return k

        def bank2():
            if bptr[0] % 2:
                bptr[0] += 1
            k = bptr[0] % 8
            bptr[0] += 2
            return k

        def BK(k):
            return pp[k // 2][:, (k % 2) * 512:(k % 2) * 512 + 512]

        def BK2(k):
            return pp[k // 2][:, :]

        def PSR(k):
            return ('ps', k)

        xsz = [('xsz', j) for j in range(NSLOT // 1024)] if phases >= 2 else []
        if phases >= 2:
            zch = newchan()
            s.op('pool', lambda e: e.memset(zt, 0.0), [], ['zt'])
            for j in range(NSLOT // 1024):
                dma('act', xs_d[j * 1024:(j + 1) * 1024, :].rearrange("(a p) d -> p a d", p=128),
                    zt.unsqueeze(1).broadcast_to([128, 8, 1024]), ['zt'], [('xsz', j)], zch)

        stg_ch = [newchan() for _ in range(NSTG_W)]
        wl = [0]

        def load_w(dst, src, ncols, gain, tag):
            for c in range(8):
                j = wl[0] % NSTG_W
                wl[0] += 1
                st = stage[j][:, 0:ncols]
                dma('sp', st, src[c * 128:(c + 1) * 128, :], [], [('stage', j)], stg_ch[j])
                eng = ['act', 'dve'][wl[0] % 2]
                if gain is None:
                    if eng == 'act':
                        act(dst[:, c, :], st, AF.Copy, [('stage', j)], [tag])
                    else:
                        cp(eng, dst[:, c, :], st, [('stage', j)], [tag])
                elif eng == 'act':
                    act(dst[:, c, :], st, AF.Copy, [('stage', j), 'consts'], [tag], scale=gain[:, c:c + 1])
                else:
                    ts(eng, dst[:, c, :], st, gain[:, c:c + 1], None, ALU.mult, None, [('stage', j), 'consts'], [tag])

        load_w(w_ck, dr["w_ck"], 1024, gmem, 'w_ck')
        load_w(w_cv, dr["w_cv"], 1024, gmem, 'w_cv')
        load_w(w_in, dr["w_in"], 1792, gmix, 'w_in')
        load_w(w_out, dr["w_out"], 1024, gout, 'w_out')
        load_w(w_cq, dr["w_cq"], 1024, gcross, 'w_cq')
        load_w(w_co, dr["w_co"], 1024, None, 'w_co')
        wr_raw = stage[0][:, 0:288].rearrange("p (c n) -> p c n", c=8)
        dma('sp', wr_raw[:, :, 0:4], dr["w_router_group"].rearrange("(c p) n -> p c n", p=128), [], [('stage', 0)], stg_ch[0])
        for g in range(4):
            dma('sp', wr_raw[:, :, 4 + 8 * g:12 + 8 * g], dr["w_router_expert"][g].rearrange("(c p) n -> p c n", p=128),
                [], [('stage', 0)], stg_ch[0])
        tt('dve', wr, wr_raw, gffn.unsqueeze(2).broadcast_to([128, 8, 36]), ALU.mult,
           [('stage', 0), 'consts'], ['wr'])
        sw_f = stage[1][:, 0:1024].rearrange("p (g j) -> p g j", g=8)
        dma('sp', sw_f, dr["sgu_w"].rearrange("g i j -> i g j"), [], [('stage', 1)], stg_ch[1])
        cp('dve', m_b.rearrange("p (g j) -> p g j", g=8), sw_f, [('stage', 1)], ['m_b'])
        k0 = bank1()
        q0b = BK(k0).bitcast(BF16)
        for g in range(8):
            tr(q0b[:, g * 128:(g + 1) * 128], m_b[:, g * 128:(g + 1) * 128], ident, ['m_b', 'consts'], [PSR(k0)])
        cp('dve', wT.rearrange("p g i -> p (g i)"), q0b, [PSR(k0)], ['wT'])
        s.op('pool', lambda e: e.affine_select(out=wT, in_=wT, pattern=[[0, 8], [1, 128]], compare_op=ALU.is_ge,
                                               fill=0.0, base=0, channel_multiplier=-1), ['wT'], ['wT'])

        def transpose8(src_bf, dstT, src_reg, dst_reg, evac_eng):
            k = bank1()
            bk = BK(k).bitcast(BF16)
            srcs = list(src_reg) if isinstance(src_reg, list) else [src_reg]
            dsts = list(dst_reg) if isinstance(dst_reg, list) else [dst_reg]
            for c in range(8):
                tr(bk[:, c * 128:(c + 1) * 128], src_bf[:, c * 128:(c + 1) * 128], ident, srcs + ['consts'], [PSR(k)])
            dst2 = dstT.rearrange("p c n -> p (c n)")
            if evac_eng == 'act':
                act(dst2, bk, AF.Copy, [PSR(k)], dsts)
            else:
                cp(evac_eng, dst2, bk, [PSR(k)], dsts)

        def linear1024(srcT, w, wtag, src_reg):
            k = bank2()
            bank = BK2(k)
            for h in range(2):
                for c in range(8):
                    mm(bank[:, h * 512:(h + 1) * 512], srcT[:, c, :], w[:, c, h * 512:(h + 1) * 512], c == 0, c == 7,
                       [src_reg, wtag], [PSR(k + h)])
            return bank, [PSR(k), PSR(k + 1)]

        P0 = pbs[0]
        mch = newchan()
        for mt in range(2):
            R = lambda n: ('m', n)
            dma('sp', m_f, mem_d[mt * 128:(mt + 1) * 128, :], [], [R('f')], mch)
            act(m_sq, m_f, AF.Square, [R('f')], [R('sq'), R('ss')], accum=P0.sm[:, 0:1])
            rstd_of(P0.sm[:, 1:2], P0.sm[:, 0:1], 1024, 'mr', [R('ss')], [R('rstd')], P0.sm[:, 2:3])
            ts('dve', m_b, m_f, P0.sm[:, 1:2], None, ALU.mult, None, [R('f'), R('rstd')], ['m_b'])
            transpose8(m_b, m_T, 'm_b', 'm_T', 'dve')
            bank, bregs = linear1024(m_T, w_ck, 'w_ck', 'm_T')
            act(m_sq, bank, AF.Square, bregs, [R('sq')])
            red(P0.sm[:, 4:8], m_sq.rearrange("p (h d) -> p h d", h=4), ALU.add, [R('sq')], [R('ssk')])
            rstd_of(P0.sm[:, 8:12], P0.sm[:, 4:8], 256, 'mk', [R('ssk')], [R('rk')], P0.sm[:, 12:16])
            tt('dve', m_b.rearrange("p (h d) -> p h d", h=4), bank.rearrange("p (h d) -> p h d", h=4),
               P0.sm[:, 8:12].unsqueeze(2).broadcast_to([128, 4, 256]), ALU.mult, bregs + [R('rk')], ['m_b'])
            kk = bank1()
            bk = BK(kk).bitcast(BF16)
            for c in range(8):
                tr(bk[:, c * 128:(c + 1) * 128], m_b[:, c * 128:(c + 1) * 128], ident, ['m_b', 'consts'], [PSR(kk)])
            for c in range(8):
                ts('dve', kcT[:, c, mt * 128:(mt + 1) * 128], bk[:, c * 128:(c + 1) * 128], gckq[:, (c % 2):(c % 2) + 1], None,
                   ALU.mult, None, [PSR(kk), 'consts'], ['kcT'])
            bank, bregs = linear1024(m_T, w_cv, 'w_cv', 'm_T')
            act(vc[:, mt, :], bank, AF.Copy, bregs, ['vc'])
        if phases >= 2:
            s.fence(skip_chans=(zch,), keep=xsz)
        else:
            s.fence()

        xch = [newchan() for _ in range(NSLOTS_T)]
        och = [newchan() for _ in range(NSLOTS_T)]
        sch = [newchan() for _ in range(NSLOTS_T)]
        for P in pbs:
            s.op('pool', lambda e, P=P: e.memset(P.vaug, 1.0), [], [('vaug', P.b)])

        def tile_gen(i):
            P = pbs[i % NSLOTS_T]
            Pp = pbs[(i - 1) % NSLOTS_T]
            b = P.b
            bp = Pp.b
            R = lambda n: (n, b)
            sm = P.sm
            rows = slice(i * 128, (i + 1) * 128)
            UG = [R('u'), R('g')]
            PTALL = [R(('PT', k)) for k in range(4)]
            dma('sp', P.xres, x_d[rows, :], [], [R('xres')], xch[b])
            act(P.sq, P.xres, AF.Square, [R('xres')], [R('sq'), R('ss1')], accum=sm[:, 0:1])
            rstd_of(sm[:, 1:2], sm[:, 0:1], 1024, 'r1%d' % b, [R('ss1')], [R('rstd1')], sm[:, 2:3])
            act(P.tb, P.xres, AF.Copy, [R('xres')], [R('tb'), R('tbh')])
            transpose8(P.tb, P.tT, [R('tb'), R('tbh')], R('tT'), 'dve')
            yield
            groups = [(0, 512), (512, 256), (768, 512), (1280, 512)]
            kq, kkv, ku, kg = bank1(), bank1(), bank1(), bank1()
            for kb, (c0_, w_) in zip((kq, kkv, ku, kg), groups):
                for c in range(8):
                    mm(BK(kb)[:, 0:w_], P.tT[:, c, :], w_in[:, c, c0_:c0_ + w_], c == 0, c == 7, [R('tT'), 'w_in'], [PSR(kb)])
            yield
            act(P.sq[:, 0:512], BK(kq), AF.Square, [PSR(kq), R('rstd1')], [R('sq')], scale=sm[:, 1:2])
            act(P.sq[:, 512:640], BK(kkv)[:, 0:128], AF.Square, [PSR(kkv), R('rstd1')], [R('sqk')], scale=sm[:, 1:2])
            red(sm[:, 16:26], P.sq[:, 0:640].rearrange("p (h d) -> p h d", d=64), ALU.add, [R('sq'), R('sqk')], [R('ssqk')])
            rstd_fast(sm[:, 32:42], sm[:, 16:26], 64, 'rqk%d' % b, [R('ssqk')], [R('rqk')], sm[:, 48:58])
            ts('dve', sm[:, 32:42], sm[:, 32:42], sm[:, 1:2], None, ALU.mult, None, [R('rqk'), R('rstd1')], [R('rqk')])
            tt('dve', P.qk[:, 0:512].rearrange("p (h d) -> p h d", d=64), BK(kq).rearrange("p (h d) -> p h d", d=64),
               sm[:, 32:40].unsqueeze(2).broadcast_to([128, 8, 64]), ALU.mult, [PSR(kq), R('rqk')], [R('qk')])
            tt('dve', P.qk[:, 512:640].rearrange("p (h d) -> p h d", d=64), BK(kkv)[:, 0:128].rearrange("p (h d) -> p h d", d=64),
               sm[:, 40:42].unsqueeze(2).broadcast_to([128, 2, 64]), ALU.mult, [PSR(kkv), R('rqk')], [R('qk')])
            act(P.vaug[:, :, 0:64], BK(kkv)[:, 128:256].rearrange("p (h d) -> p h d", d=64), AF.Copy, [PSR(kkv), R('rstd1')],
                [('vaug', b)], scale=sm[:, 1:2])
            act(P.ug[:, 0:512], BK(ku), AF.Gelu_apprx_tanh, [PSR(ku), R('rstd1')], [R('u')], scale=sm[:, 1:2])
            act(P.ug[:, 512:1024], BK(kg), AF.Gelu_apprx_tanh, [PSR(kg), R('rstd1')], [R('g')], scale=sm[:, 1:2])
            act(P.sq[:, 512:1024], P.ug[:, 512:1024], AF.Square, [R('g')], [R('sqk'), R('ssg')], accum=sm[:, 3:4])
            rstd_of(sm[:, 4:5], sm[:, 3:4], 512, 'rg%d' % b, [R('ssg')], [R('rg')], sm[:, 5:6])
            stt(P.gn, P.ug[:, 512:1024], sm[:, 4:5], gsgu, ALU.mult, ALU.mult, [R('g'), R('rg'), 'consts'], [R('gn')])
            yield
            kqt, kkt = bank1(), bank1()
            qTb = BK(kqt).bitcast(BF16)
            kTb = BK(kkt).bitcast(BF16)
            for h in range(8):
                tr(qTb[0:64, h * 128:(h + 1) * 128], P.qk[:, h * 64:(h + 1) * 64], ident, [R('qk'), 'consts'], [PSR(kqt)])
            for h in range(2):
                tr(kTb[0:64, h * 128:(h + 1) * 128], P.qk[:, 512 + h * 64:512 + (h + 1) * 64], ident, [R('qk'), 'consts'], [PSR(kkt)])
            cp('dve', P.qT, qTb[0:64, 0:1024], [PSR(kqt)], [R('qT')])
            ts('dve', P.kT, kTb[0:64, 0:256], gqk[0:64, 0:1], None, ALU.mult, None, [PSR(kkt), 'consts'], [('kT', b)])
            ksg = bank1()
            for g in range(8):
                mm(BK(ksg)[:, g * 64:(g + 1) * 64], wT[:, g, :], P.gn[:, g * 64:(g + 1) * 64], True, True, [R('gn'), 'wT'], [PSR(ksg)])
            yield
            order = [(0, 'prev'), (0, 'cur'), (1, 'cur'), (1, 'prev')]
            for kvh, which in order:
                if which == 'prev' and i == 0:
                    continue
                ks = bank1()
                kt = P.kT if which == 'cur' else Pp.kT
                ktreg = ('kT', b) if which == 'cur' else ('kT', bp)
                mbias = mb_cur if which == 'cur' else mb_prev
                mm(BK(ks), kt[:, kvh * 128:(kvh + 1) * 128], P.qT[:, kvh * 512:(kvh + 1) * 512], True, True,
                   [ktreg, R('qT')], [PSR(ks)])
                pidx = kvh * 2 + (0 if which == 'prev' else 1)
                act(P.PT[:, pidx, :], BK(ks), AF.Exp, [PSR(ks)], [R(('PT', pidx))], scale=0.125)
                if which == 'cur':
                    s.op('pool', lambda e, t=P.PT[:, pidx, :]: e.affine_select(
                        out=t, in_=t, pattern=[[0, 4], [1, 128]], compare_op=ALU.is_ge, fill=0.0, base=0, channel_multiplier=-1),
                        [R(('PT', pidx))], [R(('PT', pidx))], dur=1400.0)
                else:
                    s.op('pool', lambda e, t=P.PT[:, pidx, :]: e.affine_select(
                        out=t, in_=t, pattern=[[0, 4], [-1, 128]], compare_op=ALU.is_ge, fill=0.0, base=-1, channel_multiplier=1),
                        [R(('PT', pidx))], [R(('PT', pidx))], dur=1400.0)
            tt('dve', P.asg[:, 512:1024], BK(ksg), bfull, ALU.add, [PSR(ksg), 'consts'], [R('sg')])
            tt('dve', P.asg[:, 512:1024], P.asg[:, 512:1024], P.ug[:, 0:512], ALU.mult, [R('sg'), R('u')], [R('sg')])
            yield
            kov = [bank1(), bank1()]
            for kvh in range(2):
                ov = BK(kov[kvh])[:, 0:260].rearrange("p (h d) -> p h d", d=65)
                for r in range(4):
                    whs = ['cur'] if i == 0 else ['prev', 'cur']
                    for wi, which in enumerate(whs):
                        pidx = kvh * 2 + (0 if which == 'prev' else 1)
                        va = P.vaug if which == 'cur' else Pp.vaug
                        vreg = ('vaug', b) if which == 'cur' else ('vaug', bp)
                        mm(ov[:, r, :], P.PT[:, pidx, r * 128:(r + 1) * 128], va[:, kvh, 0:65], wi == 0, wi == len(whs) - 1,
                           [R(('PT', pidx)), vreg], [PSR(kov[kvh])])
            for kvh in range(2):
                ov = BK(kov[kvh])[:, 0:260].rearrange("p (h d) -> p h d", d=65)
                tt('dve', sm[:, 64 + kvh * 4:68 + kvh * 4].unsqueeze(2), ov[:, :, 64:65], esink[:, kvh * 4:kvh * 4 + 4].unsqueeze(2), ALU.add,
                   [PSR(kov[kvh]), 'consts'], [R(('den', kvh))])
                s.op('dve', lambda e, o=sm[:, 72 + kvh * 4:76 + kvh * 4], i_=sm[:, 64 + kvh * 4:68 + kvh * 4]: e.reciprocal(out=o, in_=i_),
                     [R(('den', kvh))], [R(('rden', kvh))])
                tt('dve', P.asg[:, kvh * 256:(kvh + 1) * 256].rearrange("p (h d) -> p h d", d=64), ov[:, :, 0:64],
                   sm[:, 72 + kvh * 4:76 + kvh * 4].unsqueeze(2).broadcast_to([128, 4, 64]), ALU.mult,
                   [PSR(kov[kvh]), R(('rden', kvh))], [R(('attn', kvh))])
            act(P.sq[:, 0:512], P.asg[:, 0:512], AF.Square, [R(('attn', 0)), R(('attn', 1))], [R('sq'), R('ssa')], accum=sm[:, 6:7])
            act(P.sq[:, 512:1024], P.asg[:, 512:1024], AF.Square, [R('sg')], [R('sqk'), R('sssg')], accum=sm[:, 7:8])
            rstd_fast(sm[:, 8:10], sm[:, 6:8], 512, 'ro%d' % b, [R('ssa'), R('sssg')], [R('ro')], sm[:, 10:12])
            ts('dve', P.tb[:, 0:512], P.asg[:, 0:512], sm[:, 8:9], None, ALU.mult, None, [R(('attn', 0)), R(('attn', 1)), R('ro')], [R('tb')])
            act(P.tb[:, 512:1024], P.asg[:, 512:1024], AF.Copy, [R('sg'), R('ro')], [R('tbh')], scale=sm[:, 9:10])
            yield
            transpose8(P.tb, P.tT, [R('tb'), R('tbh')], R('tT'), 'act')
            yield
            bank, bregs = linear1024(P.tT, w_out, 'w_out', R('tT'))
            tt('dve', P.xres, bank, P.xres, ALU.add, bregs + [R('xres')], [R('xres')])
            yield
            act(P.sq, P.xres, AF.Square, [R('xres')], [R('sq'), R('sqk'), R('ss2')], accum=sm[:, 12:13])
            rstd_of(sm[:, 13:14], sm[:, 12:13], 1024, 'r2%d' % b, [R('ss2')], [R('rstd2')], sm[:, 14:15])
            cp('dve', P.tb, P.xres, [R('xres')], [R('tb'), R('tbh')])
            transpose8(P.tb, P.tT, [R('tb'), R('tbh')], R('tT'), 'act')
            yield
            bank, bregs = linear1024(P.tT, w_cq, 'w_cq', R('tT'))
            yield
            act(P.sq, bank, AF.Square, bregs + [R('rstd2')], [R('sq'), R('sqk')], scale=sm[:, 13:14])
            red(sm[:, 80:84], P.sq.rearrange("p (h d) -> p h d", h=4), ALU.add, [R('sq'), R('sqk')], [R('ssqc')])
            rstd_fast(sm[:, 84:88], sm[:, 80:84], 256, 'rqc%d' % b, [R('ssqc')], [R('rqc')], sm[:, 88:92])
            ts('dve', sm[:, 84:88], sm[:, 84:88], sm[:, 13:14], None, ALU.mult, None, [R('rqc'), R('rstd2')], [R('rqc')])
            tt('dve', P.tb.rearrange("p (h d) -> p h d", h=4), bank.rearrange("p (h d) -> p h d", h=4),
               sm[:, 84:88].unsqueeze(2).broadcast_to([128, 4, 256]), ALU.mult, bregs + [R('rqc')], [R('tb'), R('tbh')])
            transpose8(P.tb, P.tT, [R('tb'), R('tbh')], R('tT'), 'act')
            yield
            ksc = bank2()
            for h in range(4):
                for mc in range(2):
                    idx = h * 2 + mc
                    for dc in range(2):
                        mm(BK2(ksc)[:, idx * 128:(idx + 1) * 128], kcT[:, h * 2 + dc, mc * 128:(mc + 1) * 128], P.tT[:, h * 2 + dc, :],
                           dc == 0, dc == 1, ['kcT', R('tT')], [PSR(ksc + idx // 4)])
            act(P.PcT.rearrange("p c n -> p (c n)"), BK2(ksc), AF.Exp, [PSR(ksc), PSR(ksc + 1)], [R('PcT')] + PTALL, scale=1.0 / 16.0)
            yield
            kpv = bank2()
            kden = bank1()
            for h in range(4):
                for mc in range(2):
                    mm(BK2(kpv)[:, h * 256:(h + 1) * 256], P.PcT[:, h * 2 + mc, :], vc[:, mc, h * 256:(h + 1) * 256], mc == 0, mc == 1,
                       [R('PcT'), 'vc'] + PTALL, [PSR(kpv + h // 2)])
            for h in range(4):
                for mc in range(2):
                    mm(BK(kden)[:, h:h + 1], P.PcT[:, h * 2 + mc, :], ones_col[:, 0:1], mc == 0, mc == 1, [R('PcT'), 'consts'] + PTALL, [PSR(kden)])
            s.op('dve', lambda e: e.reciprocal(out=sm[:, 92:96], in_=BK(kden)[:, 0:4]), [PSR(kden)], [R('rdc')])
            tt('dve', P.tb.rearrange("p (h d) -> p h d", h=4), BK2(kpv).rearrange("p (h d) -> p h d", h=4),
               sm[:, 92:96].unsqueeze(2).broadcast_to([128, 4, 256]), ALU.mult, [PSR(kpv), PSR(kpv + 1), R('rdc')], [R('tb'), R('tbh')])
            transpose8(P.tb, P.tT, [R('tb'), R('tbh')], R('tT'), 'act')
            yield
            bank, bregs = linear1024(P.tT, w_co, 'w_co', R('tT'))
            tt('dve', P.xres, bank, P.xres, ALU.add, bregs + [R('xres')], [R('xres')])
            yield
            dma('sp', out_d[rows, :], P.xres, [R('xres')], [('outd', i)], och[b])
            if phases < 2:
                yield
                return
            rt = P.rt
            act(P.sq, P.xres, AF.Square, [R('xres')], [R('sq'), R('sqk'), R('ss3')], accum=sm[:, 96:97])
            rstd_of(sm[:, 97:98], sm[:, 96:97], 1024, 'r3%d' % b, [R('ss3')], [R('rstd3')], sm[:, 98:99])
            stt(P.tb, P.xres, sm[:, 97:98], gffn_b, ALU.mult, ALU.mult, [R('xres'), R('rstd3'), 'consts'], [R('tb'), R('tbh')])
            kx = bank2()
            for c in range(8):
                tr(BK2(kx)[:, c * 128:(c + 1) * 128], P.xres[:, c * 128:(c + 1) * 128], identf, [R('xres'), 'identf'], [PSR(kx + c // 4)])
            x2Tf = P.x2T.rearrange("p c n -> p (c n)")
            act(x2Tf[:, 0:512], BK2(kx)[:, 0:512], AF.Copy, [PSR(kx)], [R('x2Ta'), R('u')])
            cp('dve', x2Tf[:, 512:1024], BK2(kx)[:, 512:1024], [PSR(kx + 1)], [R('x2Tb'), R('g')])
            yield
            kl = bank1()
            for c in range(8):
                mm(BK(kl)[:, 0:36], P.x2T[:, c, :], wr[:, c, :], c == 0, c == 7, [R('x2Ta'), R('x2Tb'), 'wr'] + UG, [PSR(kl)])
            L = rt[:, 0:36]
            ts('dve', L, BK(kl)[:, 0:36], sm[:, 97:98], None, ALU.mult, None, [PSR(kl), R('rstd3')], [R('L')])
            red(rt[:, 36:37], rt[:, 0:4], ALU.max, [R('L')], [R('mx')])
            ts('dve', rt[:, 40:44], rt[:, 0:4], rt[:, 36:37], None, ALU.is_equal, None, [R('L'), R('mx')], [R('ohg')])
            ts('dve', rt[:, 37:38], rt[:, 36:37], -1.0, None, ALU.mult, None, [R('mx')], [R('nmx')])
            act(rt[:, 44:48], rt[:, 0:4], AF.Exp, [R('L'), R('nmx')], [R('eg'), R('sumeg')], bias=rt[:, 37:38], accum=rt[:, 38:39])
            s.op('dve', lambda e: e.reciprocal(out=rt[:, 39:40], in_=rt[:, 38:39]), [R('sumeg')], [R('gw')])
            tt('dve', rt[:, 48:80].rearrange("p (g e) -> p g e", g=4), rt[:, 4:36].rearrange("p (g e) -> p g e", g=4),
               rt[:, 40:44].unsqueeze(2).broadcast_to([128, 4, 8]), ALU.mult, [R('L'), R('ohg')], [R('ml')])
            red(rt[:, 80:88], rt[:, 48:80].rearrange("p (g e) -> p e g", g=4), ALU.add, [R('ml')], [R('loc')])
            s.op('dve', lambda e: e.max(out=rt[:, 88:96], in_=rt[:, 80:88]), [R('loc')], [R('m8')])
            ts('dve', rt[:, 96:104], rt[:, 80:88], rt[:, 88:89], None, ALU.is_equal, None, [R('loc'), R('m8')], [R('oh1')])
            ts('dve', rt[:, 104:112], rt[:, 80:88], rt[:, 89:90], None, ALU.is_equal, None, [R('loc'), R('m8')], [R('oh2')])
            tt('dve', rt[:, 112:113], rt[:, 89:90], rt[:, 88:89], ALU.subtract, [R('m8')], [R('dlt')])
            act(rt[:, 113:114], rt[:, 112:113], AF.Exp, [R('dlt')], [R('e2')])
            ts('dve', rt[:, 114:115], rt[:, 113:114], 1.0, None, ALU.add, None, [R('e2')], [R('den2')])
            s.op('dve', lambda e: e.reciprocal(out=rt[:, 115:116], in_=rt[:, 114:115]), [R('den2')], [R('p1')])
            tt('dve', rt[:, 116:117], rt[:, 115:116], rt[:, 39:40], ALU.mult, [R('p1'), R('gw')], [R('g1')])
            tt('dve', rt[:, 117:118], rt[:, 116:117], rt[:, 113:114], ALU.mult, [R('g1'), R('e2')], [R('g2')])
            OH1 = rt[:, 128:160]
            OH2 = rt[:, 160:192]
            tt('dve', OH1.rearrange("p (g e) -> p g e", g=4), rt[:, 40:44].unsqueeze(2).broadcast_to([128, 4, 8]),
               rt[:, 96:104].unsqueeze(1).broadcast_to([128, 4, 8]), ALU.mult, [R('ohg'), R('oh1')], [R('OH1')])
            tt('dve', OH2.rearrange("p (g e) -> p g e", g=4), rt[:, 40:44].unsqueeze(2).broadcast_to([128, 4, 8]),
               rt[:, 104:112].unsqueeze(1).broadcast_to([128, 4, 8]), ALU.mult, [R('ohg'), R('oh2')], [R('OH2')])
            tt('dve', P.Mb, OH1, OH2, ALU.add, [R('OH1'), R('OH2')], [R('Mb')])
            yield
            kc = bank1()
            mm(BK(kc)[:, 0:32], ustrict, P.Mb, True, True, [R('Mb'), 'consts'], [PSR(kc)])
            mm(BK(kc)[:, 32:64], ones_bf, P.Mb, True, True, [R('Mb'), 'consts'], [PSR(kc)])
            posv = rt[:, 192:224]
            tt('dve', posv, BK(kc)[:, 0:32], cnt, ALU.add, [PSR(kc), 'cnt'], [R('pos')])
            tt('dve', cnt, BK(kc)[:, 32:64], cnt, ALU.add, [PSR(kc), 'cnt'], ['cnt'])
            ts('dve', rt[:, 224:256], posv, float(CAP), 1.0e6, ALU.is_ge, ALU.mult, [R('pos')], [R('ovf')])
            tt('dve', posv, posv, rt[:, 224:256], ALU.add, [R('pos'), R('ovf')], [R('pos')])
            tt('dve', posv, posv, eoff, ALU.add, [R('pos'), 'consts'], [R('pos')])
            tt('dve', rt[:, 256:288], posv, OH1, ALU.mult, [R('pos'), R('OH1')], [R('t1')])
            red(rt[:, 118:119], rt[:, 256:288], ALU.add, [R('t1')], [R('sl1')])
            tt('dve', rt[:, 288:320], posv, OH2, ALU.mult, [R('pos'), R('OH2')], [R('t2')])
            red(rt[:, 119:120], rt[:, 288:320], ALU.add, [R('t2')], [R('sl2')])
            cp('dve', slot_i[:, i, :], rt[:, 118:120], [R('sl1'), R('sl2')], [('slot', i)])
            ts('dve', rt[:, 120:122], rt[:, 118:120], float(NSLOT), None, ALU.is_lt, None, [R('sl1'), R('sl2')], [R('ok')])
            tt('dve', gate_f[:, i, :], rt[:, 116:118], rt[:, 120:122], ALU.mult, [R('g1'), R('g2'), R('ok')], [('gate', i)])
            for k in range(2):
                s.op('pool', lambda e, k=k: e.indirect_dma_start(
                    out=xs_d[:, :], out_offset=bass.IndirectOffsetOnAxis(ap=slot_i[:, i, k:k + 1], axis=0),
                    in_=P.tb[:, :], in_offset=None, bounds_check=bcreg(e), oob_is_err=False),
                    [R('tb'), R('tbh'), ('slot', i)] + xsz, [('xs', i, k)], chan=sch[b])
            yield

        gens = [tile_gen(i) for i in range(NT)]
        live = []
        nxt = 0
        NSTG = 18
        step = 0
        while nxt < NT or live:
            if nxt < NT and len(live) < NSLOTS_T and (not live or step % (NSTG // NSLOTS_T) == 0):
                live.append(gens[nxt])
                nxt += 1
            for g in list(live):
                try:
                    next(g)
                except StopIteration:
                    live.remove(g)
            step += 1

        if phases >= 2:
            s.fence()
            s.reschedule(1)
            ar.reset(0)
            w1b = [ar.alloc([128, 8, 512], BF16) for _ in range(3)]
            w3b = [ar.alloc([128, 8, 512], BF16) for _ in range(3)]
            w2b = [ar.alloc([128, 4, 1024], BF16) for _ in range(3)]
            xst = [ar.alloc([128, 4, 1024], BF16) for _ in range(2)]
            xsT = [ar.alloc([128, 8, 512], BF16) for _ in range(2)]
            hidT = [ar.alloc([128, 4, 512], BF16) for _ in range(2)]
            slt = [ar.alloc([128, 512], F32) for _ in range(2)]
            yo = [ar.alloc([128, 1024], BF16) for _ in range(3)]
            p3buf = ar.off
            wch = [[newchan() for _ in range(3)] for _ in range(3)]
            xsch = [newchan() for _ in range(2)]
            ych = [newchan() for _ in range(3)]
            NB = CAP // 128
            NE = NEXP if phases >= 2 else 0

            def load_weights(e):
                wb = e % 3
                dma('pool', w1b[wb], dr["w1"][e].rearrange("(c p) f -> p c f", p=128), [], [('w1', wb, 0)], wch[wb][0])
                dma('pool', w3b[wb], dr["w3"][e].rearrange("(c p) f -> p c f", p=128), [], [('w3', wb, 0)], wch[wb][1])
                dma('pool', w2b[wb], dr["w2"][e].rearrange("(c p) f -> p c f", p=128), [], [('w2', wb, 0)], wch[wb][2])

            def load_xs(e):
                eb = e % 2
                dma('sp', xst[eb], xs_d[e * CAP:(e + 1) * CAP, :].rearrange("(sb p) d -> p sb d", p=128), [], [('xst', eb)], xsch[eb])

            def do_T(e):
                eb = e % 2
                for sb in range(NB):
                    bank = pp[0][:, (sb % 2) * 512:(sb % 2) * 512 + 512].bitcast(BF16)
                    breg = ('p2', sb % 2)
                    for c in range(8):
                        tr(bank[:, c * 128:(c + 1) * 128], xst[eb][:, sb, c * 128:(c + 1) * 128], ident, [('xst', eb), 'consts'], [breg])
                    dst = xsT[eb][:, :, sb * 128:(sb + 1) * 128]
                    src = bank.rearrange("p (c n) -> p c n", c=8)
                    if sb % 2 == 0:
                        act(dst, src, AF.Copy, [breg], [('xsT', eb, sb)])
                    else:
                        cp('dve', dst, src, [breg], [('xsT', eb, sb)])

            def do_H(e):
                eb = e % 2
                wb = e % 3
                xreg = [('xsT', eb, sb) for sb in range(NB)]
                for fc in range(4):
                    hb = pp[1 + fc % 2]
                    for c in range(8):
                        mm(hb[:, 0:512], w1b[wb][:, c, fc * 128:(fc + 1) * 128], xsT[eb][:, c, :], c == 0, c == 7,
                           xreg + [('w1', wb, 0)], [('p2h', fc % 2, 0)])
                    for c in range(8):
                        mm(hb[:, 512:1024], w3b[wb][:, c, fc * 128:(fc + 1) * 128], xsT[eb][:, c, :], c == 0, c == 7,
                           xreg + [('w3', wb, 0)], [('p2h', fc % 2, 1)])
                    act(slt[fc % 2], hb[:, 0:512], AF.Silu, [('p2h', fc % 2, 0)], [('slt', fc % 2)])
                    tt('dve', hidT[eb][:, fc, :], hb[:, 512:1024], slt[fc % 2], ALU.mult, [('p2h', fc % 2, 1), ('slt', fc % 2)], [('hidT', eb, fc)])

            ycount = [0]

            def do_Y(e):
                eb = e % 2
                wb = e % 3
                hreg = [('hidT', eb, fc) for fc in range(4)]
                for sb in range(NB):
                    ob = ycount[0] % 3
                    ycount[0] += 1
                    for hlf in range(2):
                        bank = pp[3][:, hlf * 512:(hlf + 1) * 512]
                        for fc in range(4):
                            mm(bank, hidT[eb][:, fc, sb * 128:(sb + 1) * 128], w2b[wb][:, fc, hlf * 512:(hlf + 1) * 512], fc == 0, fc == 3,
                               hreg + [('w2', wb, 0)], [('p2y', hlf)])
                        if hlf == 0:
                            act(yo[ob][:, 0:512], bank, AF.Copy, [('p2y', 0)], [('yo', ob, 0)])
                        else:
                            cp('dve', yo[ob][:, 512:1024], bank, [('p2y', 1)], [('yo', ob, 1)])
                    r0 = e * CAP + sb * 128
                    dma('sp', ys_d[r0:r0 + 128, :], yo[ob], [('yo', ob, 0), ('yo', ob, 1)], [('ys', e, sb)], ych[ob])

            load_weights(0)
            load_weights(1)
            load_xs(0)
            do_T(0)
            for e in range(NE):
                if e + 2 < NE:
                    load_weights(e + 2)
                if e + 1 < NE:
                    load_xs(e + 1)
                do_H(e)
                if e + 1 < NE:
                    do_T(e + 1)
                do_Y(e)
            s.fence()
            ar.reset(p3buf)
            cb = []
            NB3 = 4
            for b in range(NB3):
                cb.append((ar.alloc([128, 1024], F32), ar.alloc([128, 1024], BF16), ar.alloc([128, 1024], BF16)))
            gch = [[newchan(), newchan()] for _ in range(NB3)]
            lch = [newchan() for _ in range(NB3)]
            fch = [newchan() for _ in range(NB3)]
            for b in range(NB3):
                s.op('dve', lambda e, b=b: e.memset(cb[b][1], 0.0), [], [('y1', b)])
                s.op('pool', lambda e, b=b: e.memset(cb[b][2], 0.0), [], [('y2', b)])
            for i in range(NT):
                b = i % NB3
                xr, y1, y2 = cb[b]
                rows = slice(i * 128, (i + 1) * 128)
                dma('sp', xr, out_d[rows, :], [], [('xr', b)], lch[b])
                for k, yb in ((0, y1), (1, y2)):
                    s.op('pool', lambda e, k=k, yb=yb, i=i: e.indirect_dma_start(
                        out=yb[:, :], out_offset=None, in_=ys_d[:, :],
                        in_offset=bass.IndirectOffsetOnAxis(ap=slot_i[:, i, k:k + 1], axis=0),
                        bounds_check=bcreg(e), oob_is_err=False), [], [('y%d' % (k + 1), b)], chan=gch[b][k])
                stt(xr, y1, gate_f[:, i, 0:1], xr, ALU.mult, ALU.add, [('xr', b), ('y1', b)], [('xr', b)])
                stt(xr, y2, gate_f[:, i, 1:2], xr, ALU.mult, ALU.add, [('xr', b), ('y2', b)], [('xr', b)])
                dma('sp', out_d[rows, :], xr, [('xr', b)], [('outd', i)], fch[b])
        s.op('sp', lambda e: e.nop(), [('outd', i) for i in range(NT)], [])
        s.emit(block)
        stats = {e: len(s.ops[e]) for e in ENGS}
        stats['waits'] = s.nwaits
        nc._kstats = stats
    return nc


_W_NAMES = ["norm_mix", "q_norm", "k_norm", "attn_sinks", "sgu_norm", "out_norm_attn", "out_norm_sgu", "norm_cross",
            "norm_mem", "cq_norm", "ck_norm", "norm_ffn", "w_in", "w_out", "w_cq", "w_ck", "w_cv", "w_co", "sgu_w",
            "sgu_b", "w_router_group", "w_router_expert", "w1", "w3", "w2"]


def kernel(**inputs):
    x = np.ascontiguousarray(inputs["x"], dtype=np.float32)
    mem = np.ascontiguousarray(inputs["mem"], dtype=np.float32)
    shared = {k: np.ascontiguousarray(inputs[k], dtype=np.float32) for k in _W_NAMES}
    nc = build(NT=32, phases=3)
    in_maps = []
    for c in range(NCORES):
        m = dict(shared)
        m["x"] = x[c]
        m["mem"] = mem[c]
        in_maps.append(m)
    res = run_bass_kernel_spmd(nc, in_maps, core_ids=list(range(NCORES)))
    return np.stack([r["out"] for r in res.results], axis=0).astype(np.float32)
```
